# Optimizing a Trainium2 kernel written in Bass

```python
import math
import jax, jax.numpy as jnp
from jax import lax
import numpy as np

D_MODEL = 1024
BATCH = 32
SEQ = 2048
DEPTH = 1

D_MIX = D_MODEL
GLA_HEADS = 4
GLA_DV = (D_MIX // 2) // GLA_HEADS
GLA_DK = GLA_DV // 2
GLA_GATE_RANK = 16
GLA_TAU = 16.0
GLA_CHUNK = 64
MOBA_HEADS = 8
MOBA_HD = (D_MIX - GLA_HEADS * GLA_DV) // MOBA_HEADS
MOBA_BLOCK = 256
MOBA_TOPK = 3
MOBA_QCHUNK = 8
REL_BUCKETS = 32
REL_MAX_DIST = 128
D_FF = 4 * D_MODEL
N_ADA = 6
EPS = 1e-6

GLA_QK_W = GLA_HEADS * GLA_DK
GLA_V_W = GLA_HEADS * GLA_DV
MOBA_W = MOBA_HEADS * MOBA_HD
IN_WIDTHS = (GLA_QK_W, GLA_QK_W, GLA_V_W, GLA_V_W, GLA_GATE_RANK, MOBA_W, MOBA_W, MOBA_W)
D_IN = sum(IN_WIDTHS)
IN_SPLITS = [int(v) for v in np.cumsum(IN_WIDTHS)[:-1]]

kernel_name = "hybrid_gla_moba_adaln_block"


def rmsnorm(x, g):
    xf = x.astype(jnp.float32)
    y = xf * lax.rsqrt(jnp.mean(xf * xf, axis=-1, keepdims=True) + EPS)
    return y.astype(x.dtype) * g


def modulate(h, shift, scale):
    return h * (1.0 + scale[:, None, :]) + shift[:, None, :]


def gla_mixer(q, k, v, log_a):
    B, S, H, dk = q.shape
    dv = v.shape[-1]
    C = GLA_CHUNK
    N = S // C
    f32 = jnp.float32

    def chunks(t):
        return t.astype(f32).reshape(B, N, C, H, t.shape[-1]).transpose(0, 3, 1, 2, 4)

    q = chunks(q) * (dk ** -0.5)
    k = chunks(k)
    v = chunks(v)
    g = chunks(log_a)
    b = jnp.cumsum(g, axis=3)
    b_last = b[:, :, :, -1:, :]
    b_ref = b[:, :, :, C // 2 - 1:C // 2, :]
    att = jnp.einsum('bhnid,bhnjd->bhnij', q * jnp.exp(b - b_ref), k * jnp.exp(b_ref - b))
    causal = jnp.tril(jnp.ones((C, C), dtype=bool))
    att = jnp.where(causal, att, 0.0)
    o_intra = jnp.einsum('bhnij,bhnjv->bhniv', att, v)
    kv = jnp.einsum('bhncd,bhncv->bhndv', k * jnp.exp(b_last - b), v)
    decay = jnp.exp(b_last[:, :, :, 0, :])

    def step(s_prev, inp):
        kv_n, dec_n = inp
        return dec_n[..., None] * s_prev + kv_n, s_prev

    s0 = jnp.zeros((B, H, dk, dv), f32)
    _, s_before = lax.scan(step, s0, (kv.transpose(2, 0, 1, 3, 4), decay.transpose(2, 0, 1, 3)))
    s_before = s_before.transpose(1, 2, 0, 3, 4)
    o_inter = jnp.einsum('bhncd,bhndv->bhncv', q * jnp.exp(b), s_before)
    o = o_intra + o_inter
    return o.transpose(0, 2, 3, 1, 4).reshape(B, S, H, dv)


def t5_bucket(rel):
    n = jnp.maximum(rel, 0)
    max_exact = REL_BUCKETS // 2
    ratio = jnp.maximum(n, 1).astype(jnp.float32) / max_exact
    large = max_exact + (jnp.log(ratio) / math.log(REL_MAX_DIST / max_exact)
                         * (REL_BUCKETS - max_exact)).astype(jnp.int32)
    large = jnp.minimum(large, REL_BUCKETS - 1)
    return jnp.where(n < max_exact, n, large)


def moba_mixer(q, k, v, rel_bias):
    B, S, H, hd = q.shape
    BLK = MOBA_BLOCK
    QC = MOBA_QCHUNK
    NB = -(-S // BLK)
    Sp = NB * BLK
    topk = min(MOBA_TOPK, NB)
    f32 = jnp.float32
    qh = q.transpose(0, 2, 1, 3) * (hd ** -0.5)
    pad = ((0, 0), (0, 0), (0, Sp - S), (0, 0))
    kb = jnp.pad(k.transpose(0, 2, 1, 3), pad).reshape(B, H, NB, BLK, hd)
    vb = jnp.pad(v.transpose(0, 2, 1, 3), pad).reshape(B, H, NB, BLK, hd)
    k_mean = jnp.mean(kb.astype(f32), axis=3)
    bias_flat = rel_bias.T.reshape(-1)
    head_off = (jnp.arange(H, dtype=jnp.int32) * REL_BUCKETS)[None, :, None, None]
    blk_pos = jnp.arange(BLK, dtype=jnp.int32)
    blk_ids = jnp.arange(NB, dtype=jnp.int32)
    nq = S // QC
    q_chunks = qh.reshape(B, H, nq, QC, hd).transpose(2, 0, 1, 3, 4)
    gather_blocks = jax.vmap(jax.vmap(lambda kk, ii: kk[ii]))

    def one_chunk(args):
        ci, q_c = args
        t = ci * QC + jnp.arange(QC, dtype=jnp.int32)
        own = (ci * QC) // BLK
        gate = jnp.einsum('bhqd,bhnd->bhqn', q_c.astype(f32), k_mean)
        gate = jnp.where(blk_ids < own, gate, -jnp.inf)
        gval, gidx = lax.top_k(gate, topk)
        valid = jnp.isfinite(gval)
        k_sel = gather_blocks(kb, gidx)
        v_sel = gather_blocks(vb, gidx)
        s_sel = jnp.einsum('bhqd,bhqkld->bhqkl', q_c, k_sel).astype(f32)
        pos_sel = gidx[..., None] * BLK + blk_pos
        rel_sel = t[None, None, :, None, None] - pos_sel
        s_sel = s_sel + bias_flat[head_off[..., None] + t5_bucket(rel_sel)]
        s_sel = jnp.where(valid[..., None], s_sel, -jnp.inf)
        k_own = lax.dynamic_slice_in_dim(kb, own, 1, axis=2)[:, :, 0]
        v_own = lax.dynamic_slice_in_dim(vb, own, 1, axis=2)[:, :, 0]
        s_own = jnp.einsum('bhqd,bhld->bhql', q_c, k_own).astype(f32)
        rel_own = t[:, None] - (own * BLK + blk_pos)[None, :]
        s_own = s_own + bias_flat[head_off + t5_bucket(rel_own)[None, None]]
        s_own = jnp.where(rel_own[None, None] >= 0, s_own, -jnp.inf)
        logits = jnp.concatenate([s_sel.reshape(B, H, QC, topk * BLK), s_own], axis=-1)
        p = jax.nn.softmax(logits, axis=-1)
        p_sel = p[..., :topk * BLK].reshape(B, H, QC, topk, BLK).astype(v.dtype)
        p_own = p[..., topk * BLK:].astype(v.dtype)
        return (jnp.einsum('bhqkl,bhqkld->bhqd', p_sel, v_sel)
                + jnp.einsum('bhql,bhld->bhqd', p_own, v_own))

    out = lax.map(one_chunk, (jnp.arange(nq, dtype=jnp.int32), q_chunks))
    return out.transpose(1, 0, 3, 2, 4).reshape(B, S, H, hd)


def setup_inputs(seed: int = 0) -> dict:
    key = jax.random.key(seed)
    ks = jax.random.split(key, 16)
    f32 = jnp.float32
    nrm = lambda k, shape, s: (jax.random.normal(k, shape, f32) * s)
    gain = lambda k, shape: 1.0 + 0.05 * jax.random.normal(k, shape, f32)
    return {
        "x": nrm(ks[0], (BATCH, SEQ, D_MODEL), 1.0),
        "c": nrm(ks[1], (BATCH, D_MODEL), 1.0),
        "w_ada": nrm(ks[2], (DEPTH, D_MODEL, N_ADA * D_MODEL), D_MODEL ** -0.5),
        "b_ada": nrm(ks[3], (DEPTH, N_ADA * D_MODEL), 0.02),
        "g_mix": gain(ks[4], (DEPTH, D_MODEL)),
        "w_in": nrm(ks[5], (DEPTH, D_MODEL, D_IN), D_MODEL ** -0.5),
        "w_gla_gate": nrm(ks[6], (DEPTH, GLA_GATE_RANK, GLA_QK_W), GLA_GATE_RANK ** -0.5),
        "b_gla_gate": nrm(ks[7], (DEPTH, GLA_QK_W), 0.1),
        "g_gla_out": gain(ks[8], (DEPTH, GLA_V_W)),
        "rel_bias": nrm(ks[9], (REL_BUCKETS, MOBA_HEADS), 0.5),
        "w_out": nrm(ks[10], (DEPTH, D_MIX, D_MODEL), D_MIX ** -0.5),
        "g_mlp": gain(ks[11], (DEPTH, D_MODEL)),
        "w_ff1": nrm(ks[12], (DEPTH, D_MODEL, D_FF), D_MODEL ** -0.5),
        "w_ff2": nrm(ks[13], (DEPTH, D_FF, D_MODEL), D_FF ** -0.5),
        "g_final": gain(ks[14], (D_MODEL,)),
    }


def reference(x, c, w_ada, b_ada, g_mix, w_in, w_gla_gate, b_gla_gate, g_gla_out, rel_bias,
              w_out, g_mlp, w_ff1, w_ff2, g_final):
    B, S, _ = x.shape
    c_act = jax.nn.silu(c)
    for l in range(DEPTH):
        ada = c_act @ w_ada[l] + b_ada[l]
        shift_a, scale_a, gate_a, shift_m, scale_m, gate_m = jnp.split(ada, N_ADA, axis=-1)
        h = modulate(rmsnorm(x, g_mix[l]), shift_a, scale_a)
        proj = h @ w_in[l]
        gq, gk, gv, gr, gg, mq, mk, mv = jnp.split(proj, IN_SPLITS, axis=-1)
        log_a = jax.nn.log_sigmoid((gg @ w_gla_gate[l] + b_gla_gate[l]).astype(jnp.float32)) / GLA_TAU
        o_gla = gla_mixer(gq.reshape(B, S, GLA_HEADS, GLA_DK), gk.reshape(B, S, GLA_HEADS, GLA_DK),
                          gv.reshape(B, S, GLA_HEADS, GLA_DV), log_a.reshape(B, S, GLA_HEADS, GLA_DK))
        o_gla = rmsnorm(o_gla.astype(x.dtype), g_gla_out[l].reshape(GLA_HEADS, GLA_DV))
        o_gla = o_gla.reshape(B, S, GLA_V_W) * jax.nn.silu(gr)
        o_moba = moba_mixer(mq.reshape(B, S, MOBA_HEADS, MOBA_HD), mk.reshape(B, S, MOBA_HEADS, MOBA_HD),
                            mv.reshape(B, S, MOBA_HEADS, MOBA_HD), rel_bias).reshape(B, S, MOBA_W)
        y = jnp.concatenate([o_gla, o_moba], axis=-1) @ w_out[l]
        x = x + gate_a[:, None, :] * y
        h = modulate(rmsnorm(x, g_mlp[l]), shift_m, scale_m)
        x = x + gate_m[:, None, :] * (jnp.square(jax.nn.relu(h @ w_ff1[l])) @ w_ff2[l])
    return rmsnorm(x, g_final)
```

```python
import numpy as np
import concourse.bass as bass
import concourse.mybir as mybir
from concourse.bass_utils import run_bass_kernel_spmd

F32 = mybir.dt.float32
BF16 = mybir.dt.bfloat16
AF = mybir.ActivationFunctionType
ALU = mybir.AluOpType
AX = mybir.AxisListType

D = 1024
S = 2048
NCORES = 8
SEQ_PER_CORE = 4
TB = 512
NT = 4
NBLK = S // TB
DIN = 3088
DFF = 4096
BIG = 30000.0
GBIG = 10000.0
EPS = 1e-6
LT = 768
SAME_ENGINE_SYNC = True


class Op:
    __slots__ = ("idx", "eng", "fn", "deps", "dma_key", "ticket", "signal", "pos")

    def __init__(self, idx, eng, fn, dma_key):
        self.idx = idx
        self.eng = eng
        self.fn = fn
        self.deps = set()
        self.dma_key = dma_key
        self.ticket = None
        self.signal = dma_key is not None
        self.pos = None


class Prog:
    ENGS = ("pe", "act", "dve", "pool", "sp")

    def __init__(self):
        self.ops = []
        self.lastw = {}
        self.rd = {}
        self.finals = []

    def add(self, eng, fn, r=(), w=(), dma_key=None):
        op = Op(len(self.ops), eng, fn, dma_key)
        deps = op.deps
        for res in r:
            o = self.lastw.get(res)
            if o is not None:
                deps.add(o)
            if isinstance(res, tuple) and res[0] == "ps":
                for o in self.rd.get(res, ()):
                    if o.eng != eng:
                        deps.add(o)
        for res in w:
            o = self.lastw.get(res)
            if o is not None:
                deps.add(o)
            for o in self.rd.get(res, ()):
                deps.add(o)
        for res in r:
            self.rd.setdefault(res, []).append(op)
        for res in w:
            self.lastw[res] = op
            self.rd[res] = []
        deps.discard(op)
        self.ops.append(op)
        return op

    def emit(self, nc):
        for op in self.ops:
            for d in op.deps:
                if d.dma_key is None and (d.eng != op.eng or (SAME_ENGINE_SYNC and op.eng != "pe")):
                    d.signal = True
        for op in self.finals:
            op.signal = True
        cnt = {e: 0 for e in self.ENGS}
        dcnt = {}
        for op in self.ops:
            if op.dma_key is not None:
                dcnt[op.dma_key] = dcnt.get(op.dma_key, 0) + 16
                op.ticket = dcnt[op.dma_key]
            elif op.signal:
                cnt[op.eng] += 1
                op.ticket = cnt[op.eng]
        keys = sorted(dcnt.keys(), key=str)
        import contextlib
        with contextlib.ExitStack() as es:
            esem = {e: es.enter_context(nc.semaphore("e_" + e)) for e in self.ENGS}
            dsem = {k: es.enter_context(nc.semaphore("d_" + str(i))) for i, k in enumerate(keys)}
            block = es.enter_context(nc.Block())
            by_eng = {e: [op for op in self.ops if op.eng == e] for e in self.ENGS}

            def run(engname, e):
                known = {}
                for op in by_eng[engname]:
                    waits = {}
                    for d in op.deps:
                        if d.dma_key is not None:
                            sem = dsem[d.dma_key]
                        else:
                            if d.eng == op.eng and (not SAME_ENGINE_SYNC or op.eng == "pe"):
                                continue
                            sem = esem[d.eng]
                        k = id(sem)
                        if d.ticket > waits.get(k, (None, 0))[1]:
                            waits[k] = (sem, d.ticket)
                    for k, (sem, v) in waits.items():
                        if known.get(k, 0) >= v:
                            continue
                        known[k] = v
                        e.wait_ge(sem, v)
                    ins = op.fn(e)
                    if op.dma_key is not None:
                        ins.then_inc(dsem[op.dma_key], 16)
                    elif op.signal:
                        ins.then_inc(esem[op.eng], 1)
                if engname == "sp":
                    for k in keys:
                        e.wait_ge(dsem[k], dcnt[k])

            @block.tensor
            def _(e):
                run("pe", e)

            @block.scalar
            def _(e):
                run("act", e)

            @block.vector
            def _(e):
                run("dve", e)

            @block.gpsimd
            def _(e):
                run("pool", e)

            @block.sync
            def _(e):
                run("sp", e)


def _t5_bucket(rel):
    n = np.maximum(rel, 0)
    max_exact = 16
    ratio = np.maximum(n, 1).astype(np.float32) / np.float32(max_exact)
    large = max_exact + (np.log(ratio).astype(np.float32) / np.float32(np.log(128 / 16)) * np.float32(16)).astype(np.int32)
    large = np.minimum(large, 31)
    return np.where(n < max_exact, n, large)


def host_consts():
    cf = np.zeros((128, 1032), np.float32)
    cf[:, 0:128] = np.eye(128)
    cf[:, 128:256] = 1.0
    j = np.arange(128)[:, None]
    i = np.arange(128)[None, :]
    cf[:, 256:384] = np.where(j <= i, -1.0 / 16, 0.0)
    cf[:, 384:512] = np.where(j > i, -1.0 / 16, 0.0)
    gm = np.zeros((8, 8, 8), np.float32)
    for own in range(8):
        for n in range(8):
            gm[own, :, n] = GBIG if n == own else (-GBIG if n > own else 0.0)
    cf[:, 512:1024] = gm.reshape(1, 512)
    for h in range(8):
        cf[h, 1024 + h] = 1.0
    cb = np.zeros((128, 288), np.float32)
    cb[:, 0:128] = np.eye(128)
    cb[:, 128:256] = np.where(j <= i, 1.0, 0.0)
    for pr in range(4):
        cb[0:64, 256 + pr * 8 + 2 * pr] = 1.0
        cb[64:128, 256 + pr * 8 + 2 * pr + 1] = 1.0
    ohk = np.zeros((8, 8 * 128), np.float32)
    for jj in range(8):
        ohk[jj, jj * 128:(jj + 1) * 128] = 1.0
    ehot = np.zeros((33, LT), np.float32)
    u = np.arange(LT)
    rel = u - 255
    bk = _t5_bucket(rel)
    for uu in range(LT):
        if rel[uu] >= 0:
            ehot[bk[uu], uu] += 1.0
        else:
            ehot[32, uu] = -BIG
    ehot[31, :] -= 1.0
    return {"cf": cf, "cb": cb, "ohk": ohk, "ehot": ehot}


def build_nc(nseq=SEQ_PER_CORE, nblk=NBLK, dbg=None, stop_after=None):
    nc = bass.Bass("TRN2", target_bir_lowering=False)
    P = Prog()
    dbg = dbg or {}

    def din(name, shape):
        return nc.dram_tensor(name, list(shape), F32, kind="ExternalInput")

    x_d = din("x", (nseq, S, D))
    c_d = din("c", (4, D))
    w_ada_d = din("w_ada", (D, 6 * D))
    b_ada_d = din("b_ada", (48, 128))
    g_mix_d = din("g_mix", (8, 128))
    g_mlp_d = din("g_mlp", (8, 128))
    w_in_d = din("w_in", (D, DIN))
    w_gg_d = din("w_gla_gate", (16, 256))
    b_gg_d = din("b_gla_gate", (1, 256))
    g_out_d = din("g_gla_out", (1, 512))
    relb_d = din("rel_bias", (32, 8))
    w_out_d = din("w_out", (D, D))
    w_ff1_d = din("w_ff1", (D, DFF))
    w_ff2_d = din("w_ff2", (DFF, D))
    g_fin_d = din("g_final", (1, D))
    cf_d = din("cf", (128, 1032))
    cb_d = din("cb", (128, 288))
    ohk_d = din("ohk", (8, 1024))
    ehot_d = din("ehot", (33, LT))
    out_d = nc.dram_tensor("out", [nseq, S, D], F32, kind="ExternalOutput")
    dbg_t = {k: nc.dram_tensor("dbg_" + k, list(shp), F32, kind="ExternalOutput") for k, shp in dbg.items()}

    wb_in = nc.dram_tensor("wb_in", [D, DIN], BF16)
    wb_out = nc.dram_tensor("wb_out", [D, D], BF16)
    wb_1 = nc.dram_tensor("wb_1", [D, DFF], BF16)
    wb_2 = nc.dram_tensor("wb_2", [DFF, D], BF16)
    TOEPN = 128 * LT + 1024
    toep = nc.dram_tensor("toep", [8 * TOEPN], F32)

    def sb(name, shape, dt):
        return nc.alloc_sbuf_tensor("s_" + name, list(shape), dt)

    def A(t, off, dims):
        return bass.AP(t, off, [list(d) for d in dims])

    cf = sb("cf", (128, 1032), F32)
    cb = sb("cb", (128, 288), BF16)
    ohk = sb("ohk", (128, 1024), BF16)
    wslot = [sb("wslot%d" % i, (128, 4096), BF16) for i in range(2)]
    xblk = sb("xblk", (128, NT, D), F32)
    hT = sb("hT", (128, 8, TB), BF16)
    gqT = sb("gqT", (128, 2, TB), F32)
    gkT = sb("gkT", (128, 2, TB), F32)
    gktok = sb("gktok", (128, NT, 256), BF16)
    gv = sb("gv", (128, NT, 512), BF16)
    t3 = sb("t3", (128, NT, 512), BF16)
    sig = sb("sig", (128, 512), F32)
    ggT = sb("ggT", (17, TB), BF16)
    wg = sb("wg", (17, 256), BF16)
    Qt = sb("Qt", (128, 4, TB), BF16)
    Kt = sb("Kt", (128, 4, S), BF16)
    Vint = sb("Vint", (128, 16, 768), BF16)
    maskT = [sb("maskT%d" % i, (128, TB), BF16) for i in range(2)]
    attnT = sb("attnT", (128, 8, TB), BF16)
    arena = sb("arena", (128, 8192), F32)
    gate_a = sb("gate_a", (128, D), F32)
    gate_m = sb("gate_m", (128, D), F32)
    gout_bc = sb("gout_bc", (128, 512), F32)
    gfin_bc = sb("gfin_bc", (128, D), F32)
    Mown = sb("Mown", (128, 8, 256), BF16)
    Mprev = sb("Mprev", (128, 8, 112), BF16)
    b31bc = sb("b31bc", (128, 8), F32)
    Lg = sb("Lg", (128, 256), F32)
    eA = sb("eA", (128, 2, 128), F32)
    eK = sb("eK", (128, 2, 128), F32)
    eB = sb("eB", (128, 2, 128), F32)
    eC = sb("eC", (128, 256), F32)
    qAz = [sb("qAz%d" % i, (128, 2, 128), BF16) for i in range(2)]
    kA = sb("kA", (128, 2, 128), BF16)
    qBz2 = [[sb("qBz%d_%d" % (j, i), (128, 2, 128), BF16) for i in range(2)] for j in range(2)]
    kC2 = [sb("kC%d" % i, (128, 256), BF16) for i in range(2)]
    attT2 = [sb("attT%d" % i, (128, 4, 128), BF16) for i in range(2)]
    on_ = sb("on", (128, 512), BF16)
    Sf = sb("Sf", (128, 2, 128), F32)
    Sb = sb("Sb", (128, 2, 128), BF16)
    brf = sb("brf", (128, 2), F32)
    nbrf = sb("nbrf", (128, 2), F32)
    dec2 = [sb("dec%d" % i, (128, 2), F32) for i in range(2)]
    oms = sb("oms", (128, 4), F32)
    orst = sb("orst", (128, 4), F32)
    ms = sb("ms", (128, NT), F32)
    rstd = sb("rstd", (128, NT), F32)
    NPT = 4
    Pt = [sb("Pt%d" % i, (128, TB), BF16) for i in range(NPT)]
    gmx = sb("gmx", (128, 64), F32)
    m8 = sb("m8", (128, 8, 8), F32)
    selt = sb("selt", (128, 64), F32)
    mrow = sb("mrow", (128, NT, 64), BF16)
    kmsum = sb("kmsum", (128, 4, 8), F32)
    kmean = sb("kmean", (128, 4, 8), BF16)
    qn2 = sb("qn2", (8, 1), F32)
    kn2 = sb("kn2", (8, 1), F32)
    kn2r = sb("kn2r", (8, 1), F32)
    bnd = sb("bnd", (8, 1), F32)
    bnd8 = sb("bnd8", (8, 8), F32)
    nbias = sb("nbias", (128, 8), F32)
    cactT = sb("cactT", (128, 32), F32)
    b48 = sb("b48", (48, 128), F32)
    g8 = sb("g8", (8, 2, 128), F32)
    badaT = sb("badaT", (128, 48), F32)
    gT = sb("gT", (128, 2, 8), F32)
    adaT = sb("adaT", (128, 48, 4), F32)
    G1T = sb("G1T", (128, 8, 4), F32)
    G2T = sb("G2T", (128, 8, 4), F32)
    diag1 = sb("diag", (128, 128), F32)
    diag = [diag1, diag1]
    rb33 = sb("rb33", (33, 8), F32)
    rbx = sb("rbx", (33, 128), F32)

    ab = arena[:].bitcast(BF16)
    uT = ab.rearrange("p (c t) -> p c t", t=TB)
    sq = ab[:, 0:4 * TB].rearrange("p (a t) -> p a t", t=TB)
    xsfull = ab[:, 8 * TB:16 * TB].rearrange("p (t d) -> p t d", d=D)
    junk = ab[:, 30 * TB:32 * TB]
    wst = arena[:, 0:4096].rearrange("p (s k n) -> p s k n", s=2, k=8)
    Gtabs = [arena[:, 4096:4096 + LT], arena[:, 4864:4864 + LT]]
    ehot_sb = arena[0:33, 5632:5632 + LT]
    c4 = arena[0:4, 6400:7424]
    c4e = gate_m[0:4, :]
    rden1 = sb("rden1", (128, TB), F32)
    rden2 = [rden1, rden1]
    ob = [sb("ob%d" % i, (128, D), F32) for i in range(2)]
    XS_RES = [[("uT", 8 + 2 * t_), ("uT", 9 + 2 * t_)] for t_ in range(NT)]
    JUNK_RES = [("uT", 30), ("uT", 31)]
    SQ_RES = [("uT", c_) for c_ in range(4)]

    NB_RING = 6
    ps = [nc.alloc_psum_tensor("ps%d" % i, [128, 512], F32) for i in range(8)]
    class Ring:
        def __init__(self, banks):
            self.banks = list(banks)
            self.i = 0

        def reset(self):
            self.i = 0

    ringD = Ring(range(6))
    ringG = Ring([0])
    ringB = Ring([1, 2])
    ringM = Ring([2, 3, 4, 5])

    def nbank(ring=None):
        ring = ring or ringD
        k = ring.banks[ring.i % len(ring.banks)]
        ring.i += 1
        return k

    ident_f = cf[:, 0:128]
    ones_f = cf[:, 128:256]
    ident_b = cb[:, 0:128]

    def dump(name, src_ap, res):
        if name in dbg_t:
            t = dbg_t[name]
            P.add("pool", lambda e: e.dma_start(out=t.ap(), in_=src_ap), r=res, w=[("dbg", name)], dma_key=("dbg", name))

    P.add("sp", lambda e: e.dma_start(out=cf[:], in_=cf_d.ap()[:, :]), w=["cf"], dma_key="cf")
    P.add("pool", lambda e: e.dma_start(out=cb[:], in_=cb_d.ap()[:, :]), w=["cb"], dma_key="cb")
    P.add("pool", lambda e: e.dma_start(out=ohk[0:8, :], in_=ohk_d.ap()[:, :]), w=["ohk"], dma_key="ohk")
    P.add("pool", lambda e: e.dma_start(out=ohk[64:72, :], in_=ohk_d.ap()[:, :]), w=["ohk2"], dma_key="ohk2")
    P.add("pool", lambda e: e.dma_start(out=wg[0:16, :], in_=w_gg_d.ap()[:, :]), w=["wg0"], dma_key="wg0")
    P.add("pool", lambda e: e.dma_start(out=wg[16:17, :], in_=b_gg_d.ap()[:, :]), w=["wg1"], dma_key="wg1")
    WB_RES = {}
    def cast_weights(group, after):
        for (nm, dst, src, rows) in group:
            nsp = 4
            rr = rows // nsp
            WB_RES[nm] = []
            for q in range(nsp):
                key = (nm, q)
                P.add("pool", lambda e, dst=dst, src=src, q=q, rr=rr: e.dma_start(
                    out=dst.ap()[q * rr:(q + 1) * rr, :], in_=src.ap()[q * rr:(q + 1) * rr, :]),
                    r=after, w=[key], dma_key=key)
                WB_RES[nm].append(key)

    cast_weights((("wb_in", wb_in, w_in_d, D), ("wb_out", wb_out, w_out_d, D)), [])

    P.add("sp", lambda e: e.dma_start(out=gout_bc[:], in_=A(g_out_d, 0, [[0, 128], [1, 512]])), w=["gout_bc"], dma_key="gout")
    P.add("sp", lambda e: e.dma_start(out=gfin_bc[:], in_=A(g_fin_d, 0, [[0, 128], [1, D]])), w=["gfin_bc"], dma_key="gfin")
    P.add("sp", lambda e: e.dma_start(out=b31bc[:], in_=A(relb_d, 31 * 8, [[0, 128], [1, 8]])), w=["b31bc"], dma_key="b31")
    P.add("sp", lambda e: e.dma_start(out=rb33[0:32, :], in_=relb_d.ap()[:, :]), w=["rb33a"], dma_key="rb33")
    P.add("sp", lambda e: e.dma_start(out=c4, in_=c_d.ap()[:, :]), w=["c4"], dma_key="c4")
    P.add("sp", lambda e: e.dma_start(out=b48[:], in_=b_ada_d.ap()[:, :]), w=["b48"], dma_key="b48")
    P.add("sp", lambda e: e.dma_start(out=g8[:, 0, :], in_=g_mix_d.ap()[:, :]), w=["g8a"], dma_key="g8a")
    P.add("sp", lambda e: e.dma_start(out=g8[:, 1, :], in_=g_mlp_d.ap()[:, :]), w=["g8b"], dma_key="g8b")
    P.add("sp", lambda e: e.dma_start(out=ehot_sb, in_=ehot_d.ap()[:, :]), w=["ehot"], dma_key="ehot")

    P.add("dve", lambda e: e.memset(ggT[:], 1.0), w=["ggT"])
    P.add("dve", lambda e: e.memset(Vint[:], 1.0), w=[("V", t_) for t_ in range(16)])
    P.add("dve", lambda e: e.memset(kmsum[:], 0.0), w=["kmsum"])
    for i_ in range(2):
        P.add("dve", lambda e, i_=i_: e.memset(qAz[i_][:], 0.0), w=[("qA", i_)])
        for j_ in range(2):
            P.add("dve", lambda e, i_=i_, j_=j_: e.memset(qBz2[j_][i_][:], 0.0), w=[("qB", j_, i_)])
    P.add("dve", lambda e: e.memset(kmean[:], 0.0), w=["kmean"])
    P.add("dve", lambda e: e.memset(rb33[32:33, :], 1.0), w=["rb33b"])

    P.add("act", lambda e: e.activation(out=c4e, in_=c4, func=AF.Exp, scale=-1.0), r=["c4"], w=["c4e"])
    P.add("dve", lambda e: e.tensor_scalar(out=c4e, in0=c4e, scalar1=1.0, scalar2=None, op0=ALU.add), r=["c4e"], w=["c4e"])
    P.add("dve", lambda e: e.reciprocal(out=c4e, in_=c4e), r=["c4e"], w=["c4e"])
    P.add("dve", lambda e: e.tensor_tensor(out=c4, in0=c4, in1=c4e, op=ALU.mult), r=["c4e", "c4"], w=["c4"])
    bk = nbank()
    for kc in range(8):
        P.add("pe", lambda e, kc=kc, bk=bk: e.matmul(ps[bk][:, kc * 4:kc * 4 + 4], lhsT=c4[:, kc * 128:(kc + 1) * 128],
                                                     rhs=cf[0:4, 0:4], start=True, stop=True),
              r=["c4", "cf"], w=[("ps", bk)])
    P.add("dve", lambda e, bk=bk: e.tensor_copy(out=cactT[:], in_=ps[bk][:, 0:32]), r=[("ps", bk)], w=["cactT"])
    bk = nbank()
    P.add("pe", lambda e, bk=bk: e.matmul(ps[bk][:, 0:48], lhsT=b48[0:48, :], rhs=cf[0:48, 0:48], start=True, stop=True),
          r=["b48", "cf"], w=[("ps", bk)])
    for q in range(2):
        P.add("pe", lambda e, bk=bk, q=q: e.matmul(ps[bk][:, 64 + q * 8:72 + q * 8], lhsT=g8[0:8, q, :], rhs=cf[0:8, 0:8],
                                                   start=True, stop=True),
              r=["g8a", "g8b", "cf"], w=[("ps", bk)])
    P.add("dve", lambda e, bk=bk: e.tensor_copy(out=badaT[:], in_=ps[bk][:, 0:48]), r=[("ps", bk)], w=["badaT"])
    P.add("dve", lambda e, bk=bk: e.tensor_copy(out=gT[:], in_=ps[bk][:, 64:80].rearrange("p (q c) -> p q c", c=8)),
          r=[("ps", bk)], w=["gT"])
    bk_ada = nbank()
    w_ada_v = w_ada_d.ap().rearrange("(k p) n -> p k n", p=128)
    for pc in range(24):
        sl = pc % 2
        P.add("sp", lambda e, pc=pc, sl=sl: e.dma_start(out=wst[:, sl], in_=w_ada_v[:, :, pc * 256:(pc + 1) * 256]),
              w=[("wst", sl)], dma_key=("wst", sl))
        for sub in range(2):
            blk = pc * 2 + sub
            for kc in range(8):
                P.add("pe", lambda e, sl=sl, sub=sub, kc=kc, blk=blk: e.matmul(
                    ps[bk_ada][:, blk * 4:blk * 4 + 4], lhsT=wst[:, sl, kc, sub * 128:(sub + 1) * 128],
                    rhs=cactT[:, kc * 4:kc * 4 + 4], start=(kc == 0), stop=(kc == 7)),
                    r=[("wst", sl), "cactT"], w=[("ps", bk_ada)])
    P.add("dve", lambda e: e.tensor_tensor(out=adaT[:], in0=ps[bk_ada][:, 0:192].rearrange("p (k b) -> p k b", b=4),
                                           in1=A(badaT, 0, [[48, 128], [1, 48], [0, 4]]), op=ALU.add),
          r=[("ps", bk_ada), "badaT"], w=["adaT"])
    for (GT, sc0, q) in ((G1T, 8, 0), (G2T, 32, 1)):
        P.add("dve", lambda e, GT=GT, sc0=sc0: e.tensor_scalar(out=GT[:], in0=adaT[:, sc0:sc0 + 8, :], scalar1=1.0,
                                                               scalar2=None, op0=ALU.add), r=["adaT"], w=[("G", q)])
        P.add("dve", lambda e, GT=GT, q=q: e.tensor_tensor(out=GT[:], in0=GT[:], in1=A(gT, q * 8, [[16, 128], [1, 8], [0, 4]]),
                                                           op=ALU.mult), r=[("G", q), "gT"], w=[("G", q)])
    dump("adaT", adaT[:].rearrange("p k b -> p (k b)"), ["adaT"])

    for h in range(8 if stop_after != "pro_a" else 0):
        Gtab = Gtabs[h % 2]
        gres = ("Gtab", h % 2)
        P.add("dve", lambda e, h=h: e.tensor_scalar(out=rbx[:], in0=cf[0:33, 128:256], scalar1=rb33[0:33, h:h + 1],
                                                    scalar2=None, op0=ALU.mult),
              r=["cf", "rb33a", "rb33b"], w=["rbx"])
        for half in range(2):
            b_ = nbank()
            P.add("pe", lambda e, half=half, b_=b_: e.matmul(ps[b_][:, 0:384], lhsT=rbx[0:33, :],
                                                             rhs=ehot_sb[:, half * 384:(half + 1) * 384], start=True, stop=True),
                  r=["rbx", "ehot"], w=[("ps", b_)])
            P.add("act", lambda e, half=half, b_=b_, Gtab=Gtab: e.activation(out=Gtab[:, half * 384:(half + 1) * 384],
                                                                            in_=ps[b_][:, 0:384], func=AF.Exp),
                  r=[("ps", b_)], w=[gres])
        P.add("sp", lambda e, h=h, Gtab=Gtab: e.dma_start(out=A(toep, h * TOEPN, [[LT, 128], [1, LT]]), in_=Gtab),
              r=[gres], w=[("toep", h)], dma_key=("toepw", h % 2))
        P.add("pool", lambda e, h=h: e.dma_start(out=Mown[:, h, :], in_=A(toep, h * TOEPN + 255, [[LT - 1, 128], [1, 256]])),
              r=[("toep", h)], w=[("Mown", h)], dma_key=("toepr", h))
        P.add("pool", lambda e, h=h: e.dma_start(out=Mprev[:, h, :], in_=A(toep, h * TOEPN + 383, [[LT - 1, 128], [1, 112]])),
              r=[("toep", h)], w=[("Mprev", h)], dma_key=("toepr2", h))

    cast_weights((("wb_1", wb_1, w_ff1_d, D), ("wb_2", wb_2, w_ff2_d, DFF)), list(WB_RES["wb_in"]) + list(WB_RES["wb_out"]))

    wctr = [0]

    def wload(src_ap, wb_name, k, n):
        sl = wctr[0] % 2
        wctr[0] += 1
        view = wslot[sl][:, 0:k * n].rearrange("p (k n) -> p k n", n=n)
        res = ("wslot", sl)
        P.add("sp", lambda e: e.dma_start(out=view, in_=src_ap), r=WB_RES[wb_name], w=[res], dma_key=res)
        return view, res

    wb_in_v = wb_in.ap().rearrange("(k p) n -> p k n", p=128)
    wb_out_v = wb_out.ap().rearrange("(k p) n -> p k n", p=128)
    wb_1_v = wb_1.ap().rearrange("(k p) n -> p k n", p=128)
    wb_2_v = wb_2.ap().rearrange("(k p) n -> p k n", p=128)

    def norm_stats():
        for tt in range(NT):
            P.add("act", lambda e, tt=tt: e.activation(out=junk, in_=xblk[:, tt, :], func=AF.Square, scale=1.0 / 32.0,
                                                       accum_out=ms[:, tt:tt + 1]),
                  r=[("x", tt)], w=JUNK_RES + [("ms", tt)])
            P.add("act", lambda e, tt=tt: e.activation(out=rstd[:, tt:tt + 1], in_=ms[:, tt:tt + 1], func=AF.Ln, bias=EPS, scale=1.0),
                  r=[("ms", tt)], w=[("rstd", tt)])
            P.add("act", lambda e, tt=tt: e.activation(out=rstd[:, tt:tt + 1], in_=rstd[:, tt:tt + 1], func=AF.Exp, scale=-0.5),
                  r=[("rstd", tt)], w=[("rstd", tt)])

    def norm_hT(b, which):
        GT = G1T if which == 0 else G2T
        gq_ = ("G", which)
        sh0 = 0 if which == 0 else 24
        norm_stats()
        for half in range(2):
            banks = [nbank() for _ in range(4)]
            for tt in range(NT):
                if half == 0:
                    P.add("act", lambda e, tt=tt: e.activation(out=xsfull[:, tt, :], in_=xblk[:, tt, :], func=AF.Identity,
                                                               scale=rstd[:, tt:tt + 1]),
                          r=[("x", tt), ("rstd", tt), "adaT"], w=XS_RES[tt])
                for q in range(4):
                    kc = half * 4 + q
                    P.add("pe", lambda e, tt=tt, kc=kc, bq=banks[q]: e.matmul(
                        ps[bq][:, tt * 128:(tt + 1) * 128], lhsT=xsfull[:, tt, kc * 128:(kc + 1) * 128], rhs=ident_b,
                        start=True, stop=True), r=XS_RES[tt] + ["cb"], w=[("ps", banks[q])])
            for q in range(4):
                kc = half * 4 + q
                bq = banks[q]
                if q % 2 == 0:
                    P.add("act", lambda e, kc=kc, bq=bq: e.activation(out=hT[:, kc, :], in_=ps[bq][:, :], func=AF.Identity,
                                                                     scale=GT[:, kc, b:b + 1], bias=adaT[:, sh0 + kc, b:b + 1]),
                          r=[("ps", bq), gq_, "adaT"], w=[("hT", kc)])
                else:
                    P.add("dve", lambda e, kc=kc, bq=bq: e.tensor_scalar(out=hT[:, kc, :], in0=ps[bq][:, :],
                                                                        scalar1=GT[:, kc, b:b + 1], scalar2=adaT[:, sh0 + kc, b:b + 1],
                                                                        op0=ALU.mult, op1=ALU.add),
                          r=[("ps", bq), gq_, "adaT"], w=[("hT", kc)])

    def acc_mm(out_ap, pairs, r, w):
        n = len(pairs)
        for i_, (l, rh) in enumerate(pairs):
            P.add("pe", lambda e, l=l, rh=rh, i_=i_: e.matmul(out_ap, lhsT=l, rhs=rh, start=(i_ == 0), stop=(i_ == n - 1)),
                  r=r, w=w)

    HT_ALL = [("hT", kc) for kc in range(8)]
    first = [True]

    class _Stop(Exception):
        pass

    def chk(name):
        if stop_after == name:
            raise _Stop()

    out_ops = []
    try:
      for b in range(nseq):
          if stop_after in ("prologue", "pro_a", "pro_b"):
              break
          for (gt_, blk0, nm) in ((gate_a, 16, "gate_a"), (gate_m, 40, "gate_m")):
              for half in range(2):
                  bk = nbank()
                  for q in range(4):
                      kc = half * 4 + q
                      dg = diag[kc % 2]
                      P.add("dve", lambda e, dg=dg, kc=kc, blk0=blk0, b=b: e.tensor_scalar(out=dg[:], in0=ident_f,
                                                                                     scalar1=adaT[:, blk0 + kc, b:b + 1], scalar2=None,
                                                                                     op0=ALU.mult),
                            r=["cf", "adaT"], w=[("diag", 0)])
                      P.add("pe", lambda e, dg=dg, q=q, bk=bk: e.matmul(ps[bk][:, q * 128:(q + 1) * 128], lhsT=ones_f, rhs=dg[:],
                                                                        start=True, stop=True),
                            r=[("diag", 0), "cf"], w=[("ps", bk)])
                  P.add("act", lambda e, gt_=gt_, half=half, bk=bk: e.activation(out=gt_[:, half * 512:(half + 1) * 512],
                                                                                in_=ps[bk][:, :], func=AF.Copy),
                        r=[("ps", bk), "cactT"], w=[(nm, half)])
          P.add("dve", lambda e: e.memset(Sf[:], 0.0), w=["Sf"])
          P.add("dve", lambda e: e.memset(Sb[:], 0.0), w=["Sb"])
          P.add("dve", lambda e: e.memset(kn2r[:], 0.0), w=["kn2r"])

          for T in range(nblk):
              i0, i1 = 2 * T, 2 * T + 1
              isdbg = first[0]
              first[0] = False
              for tt in range(NT):
                  xsrc = x_d.ap()[b, T * TB + tt * 128:T * TB + (tt + 1) * 128, :]
                  P.add("sp", lambda e, xsrc=xsrc, tt=tt: e.dma_start(out=xblk[:, tt, :], in_=xsrc), w=[("x", tt)],
                        dma_key=("xload", tt))
              norm_hT(b, 0)
              if isdbg:
                  dump("hT", hT[:].rearrange("p k t -> p (k t)"), HT_ALL)
              if stop_after == "norm":
                  break
              wv, wr = wload(wb_in_v[:, :, 0:512], "wb_in", 8, 512)
              for cbk in range(4):
                  bk = nbank()
                  acc_mm(ps[bk][:, :], [(wv[:, kc, cbk * 128:(cbk + 1) * 128], hT[:, kc, :]) for kc in range(8)],
                         r=[wr] + HT_ALL, w=[("ps", bk)])
                  if cbk < 2:
                      P.add("act", lambda e, cbk=cbk, bk=bk: e.activation(out=gqT[:, cbk, :], in_=ps[bk][:, :], func=AF.Identity, scale=0.125),
                            r=[("ps", bk)], w=[("gqT", cbk)])
                  else:
                      P.add("dve", lambda e, cbk=cbk, bk=bk: e.tensor_copy(out=gkT[:, cbk - 2, :], in_=ps[bk][:, :]),
                            r=[("ps", bk)], w=[("gkT", cbk - 2)])
              for tt in range(NT):
                  bk = nbank()
                  acc_mm(ps[bk][:, 0:256], [(hT[:, kc, tt * 128:(tt + 1) * 128], wv[:, kc, 256:512]) for kc in range(8)],
                         r=[wr] + HT_ALL, w=[("ps", bk)])
                  P.add("act", lambda e, tt=tt, bk=bk: e.activation(out=gktok[:, tt, :], in_=ps[bk][:, 0:256], func=AF.Copy),
                        r=[("ps", bk)], w=[("gktok", tt)])
              chk("ip0")
              wv, wr = wload(wb_in_v[:, :, 512:1024], "wb_in", 8, 512)
              for tt in range(NT):
                  bk = nbank()
                  acc_mm(ps[bk][:, :], [(hT[:, kc, tt * 128:(tt + 1) * 128], wv[:, kc, :]) for kc in range(8)],
                         r=[wr] + HT_ALL, w=[("ps", bk)])
                  P.add("dve", lambda e, tt=tt, bk=bk: e.tensor_copy(out=gv[:, tt, :], in_=ps[bk][:, :]),
                        r=[("ps", bk)], w=[("gv", tt)])
              chk("ip1")
              wv, wr = wload(wb_in_v[:, :, 1024:1536], "wb_in", 8, 512)
              for tt in range(NT):
                  bk = nbank()
                  acc_mm(ps[bk][:, :], [(hT[:, kc, tt * 128:(tt + 1) * 128], wv[:, kc, :]) for kc in range(8)],
                         r=[wr] + HT_ALL, w=[("ps", bk)])
                  P.add("act", lambda e, bk=bk: e.activation(out=sig[:], in_=ps[bk][:, :], func=AF.Exp, scale=-1.0),
                        r=[("ps", bk)], w=["sig"])
                  P.add("dve", lambda e, tt=tt, bk=bk: e.tensor_tensor(out=t3[:, tt, :], in0=ps[bk][:, :], in1=gout_bc[:], op=ALU.mult),
                        r=[("ps", bk), "gout_bc"], w=[("t3", tt)])
                  P.add("act", lambda e: e.activation(out=sig[:], in_=sig[:], func=AF.Ln, bias=1.0, scale=1.0), r=["sig"], w=["sig"])
                  P.add("act", lambda e: e.activation(out=sig[:], in_=sig[:], func=AF.Exp, scale=-1.0), r=["sig"], w=["sig"])
                  P.add("dve", lambda e, tt=tt: e.tensor_tensor(out=t3[:, tt, :], in0=t3[:, tt, :], in1=sig[:], op=ALU.mult),
                        r=["sig", ("t3", tt)], w=[("t3", tt)])
              chk("ip2")
              wvg, wrg = wload(wb_in_v[:, :, 1536:1552], "wb_in", 8, 16)
              bk = nbank()
              acc_mm(ps[bk][0:16, :], [(wvg[:, kc, 0:16], hT[:, kc, :]) for kc in range(8)], r=[wrg] + HT_ALL, w=[("ps", bk)])
              P.add("act", lambda e, bk=bk: e.activation(out=ggT[0:16, :], in_=ps[bk][0:16, :], func=AF.Copy),
                    r=[("ps", bk)], w=["ggT"])
              chk("ip3")
              wv, wr = wload(wb_in_v[:, :, 1552:2064], "wb_in", 8, 512)
              for pr in range(4):
                  bk = nbank()
                  acc_mm(ps[bk][:, :], [(wv[:, kc, pr * 128:(pr + 1) * 128], hT[:, kc, :]) for kc in range(8)],
                         r=[wr] + HT_ALL, w=[("ps", bk)])
                  P.add("act", lambda e, pr=pr, bk=bk: e.activation(out=Qt[:, pr, :], in_=ps[bk][:, :], func=AF.Identity, scale=0.125),
                        r=[("ps", bk)], w=[("Qt", pr)])
              chk("ip4")
              wv, wr = wload(wb_in_v[:, :, 2064:2576], "wb_in", 8, 512)
              for pr in range(4):
                  bk = nbank()
                  acc_mm(ps[bk][:, :], [(wv[:, kc, pr * 128:(pr + 1) * 128], hT[:, kc, :]) for kc in range(8)],
                         r=[wr] + HT_ALL, w=[("ps", bk)])
                  P.add("dve", lambda e, pr=pr, bk=bk, T=T: e.tensor_copy(out=Kt[:, pr, T * TB:(T + 1) * TB], in_=ps[bk][:, :]),
                        r=[("ps", bk)], w=[("Kt", pr, T)])
              chk("ip5")
              wv, wr = wload(wb_in_v[:, :, 2576:3088], "wb_in", 8, 512)
              for tt in range(NT):
                  g_t = T * NT + tt
                  bk = nbank()
                  acc_mm(ps[bk][:, :], [(hT[:, kc, tt * 128:(tt + 1) * 128], wv[:, kc, :]) for kc in range(8)],
                         r=[wr] + HT_ALL, w=[("ps", bk)])
                  P.add("act", lambda e, g_t=g_t, bk=bk: e.activation(
                      out=A(Vint, g_t * 768, [[16 * 768, 128], [192, 4], [1, 64]]),
                      in_=A(ps[bk], 0, [[512, 128], [128, 4], [1, 64]]), func=AF.Copy),
                      r=[("ps", bk)], w=[("V", g_t)])
                  P.add("dve", lambda e, g_t=g_t, bk=bk: e.tensor_copy(
                      out=A(Vint, g_t * 768 + 128, [[16 * 768, 128], [192, 4], [1, 64]]),
                      in_=A(ps[bk], 64, [[512, 128], [128, 4], [1, 64]])),
                      r=[("ps", bk), ("V", g_t)], w=[("V", g_t)])
              if isdbg:
                  dump("gqT", gqT[:].rearrange("p k t -> p (k t)"), [("gqT", 0), ("gqT", 1)])
                  dump("gktok", gktok[:].rearrange("p k t -> p (k t)"), [("gktok", t_) for t_ in range(NT)])
                  dump("t3", t3[:].rearrange("p k t -> p (k t)"), [("t3", t_) for t_ in range(NT)])
                  dump("Qt", Qt[:].rearrange("p k t -> p (k t)"), [("Qt", p_) for p_ in range(4)])
                  dump("Kt", Kt[:, :, 0:TB], [("Kt", p_, 0) for p_ in range(4)])
                  dump("Vint", Vint[:, 0:4, :].rearrange("p k t -> p (k t)"), [("V", t_) for t_ in range(4)])
              if stop_after == "inproj":
                  break

              def gla_gen():
                  def gla_front(tt):
                      csl = slice(tt * 128, (tt + 1) * 128)
                      bz = nbank(ringG)
                      P.add("pe", lambda e, bz=bz, csl=csl: e.matmul(ps[bz][:, 0:256], lhsT=ggT[0:17, csl], rhs=wg[0:17, :], start=True, stop=True),
                            r=["ggT", "wg0", "wg1"], w=[("ps", bz)])
                      P.add("act", lambda e, bz=bz: e.activation(out=Lg[:], in_=ps[bz][:, 0:256], func=AF.Exp, scale=-1.0),
                            r=[("ps", bz)], w=["Lg"])
                      P.add("act", lambda e: e.activation(out=Lg[:], in_=Lg[:], func=AF.Ln, bias=1.0, scale=1.0), r=["Lg"], w=["Lg"])
                      yield
                      bc_ = nbank(ringG)
                      bt_ = bc_
                      P.add("pe", lambda e, bc_=bc_: e.matmul(ps[bc_][:, 0:256], lhsT=cf[:, 384:512], rhs=Lg[:], start=True, stop=True),
                            r=["Lg", "cf"], w=[("ps", bc_)])
                      for blk in range(2):
                          P.add("pe", lambda e, bt_=bt_, blk=blk: e.matmul(ps[bt_][:, 256 + blk * 128:256 + (blk + 1) * 128],
                                                                           lhsT=Lg[:, blk * 128:(blk + 1) * 128], rhs=cf[:, 256:384],
                                                                           start=True, stop=True),
                                r=["Lg", "cf"], w=[("ps", bt_)])
                      bT3 = ps[bt_][:, 256:512].rearrange("p (k t) -> p k t", t=128)
                      P.add("dve", lambda e, bT3=bT3: e.tensor_copy(out=brf[:], in_=bT3[:, :, 63]), r=[("ps", bt_)], w=["brf"])
                      P.add("dve", lambda e, bT3=bT3: e.tensor_scalar(out=nbrf[:], in0=bT3[:, :, 63], scalar1=-1.0, scalar2=None, op0=ALU.mult),
                            r=[("ps", bt_)], w=["nbrf"])
                      P.add("act", lambda e, bT3=bT3: e.activation(out=dec2[tt % 2][:], in_=bT3[:, :, 127], func=AF.Exp), r=[("ps", bt_)], w=[("dec", tt % 2)])
                      for blk in range(2):
                          P.add("act", lambda e, blk=blk, bT3=bT3: e.activation(out=eA[:, blk, :], in_=bT3[:, blk, :], func=AF.Exp,
                                                                               bias=nbrf[:, blk:blk + 1], scale=1.0),
                                r=[("ps", bt_), "nbrf"], w=[("eA", blk)])
                          P.add("act", lambda e, blk=blk, bT3=bT3: e.activation(out=eK[:, blk, :], in_=bT3[:, blk, :], func=AF.Exp,
                                                                               bias=brf[:, blk:blk + 1], scale=-1.0),
                                r=[("ps", bt_), "brf"], w=[("eK", blk)])
                      P.add("act", lambda e, bT3=bT3: e.activation(out=eB[:], in_=bT3, func=AF.Exp), r=[("ps", bt_)], w=["eB"])
                      P.add("act", lambda e, bc_=bc_: e.activation(out=eC[:], in_=ps[bc_][:, 0:256], func=AF.Exp), r=[("ps", bc_)], w=["eC"])
                      yield
                      for par in range(2):
                          rs = slice(64 * par, 64 * par + 64)
                          P.add("dve", lambda e, csl=csl, par=par, rs=rs: e.tensor_tensor(out=qAz[par][rs, :, :], in0=gqT[rs, :, csl], in1=eA[rs, :, :], op=ALU.mult),
                                r=[("gqT", 0), ("gqT", 1), ("eA", 0), ("eA", 1)], w=[("qA", par)])
                          P.add("dve", lambda e, csl=csl, par=par, rs=rs: e.tensor_tensor(out=qBz2[tt % 2][par][rs, :, :], in0=gqT[rs, :, csl], in1=eB[rs, :, :], op=ALU.mult),
                                r=[("gqT", 0), ("gqT", 1), "eB"], w=[("qB", tt % 2, par)])
                      P.add("dve", lambda e, csl=csl: e.tensor_tensor(out=kA[:], in0=gkT[:, :, csl], in1=eK[:], op=ALU.mult),
                            r=[("gkT", 0), ("gkT", 1), ("eK", 0), ("eK", 1)], w=["kA"])
                      P.add("dve", lambda e, tt=tt: e.tensor_tensor(out=kC2[tt % 2][:], in0=gktok[:, tt, :], in1=eC[:], op=ALU.mult),
                            r=[("gktok", tt), "eC"], w=[("kC", tt % 2)])
                      yield
                      ba = nbank(ringG)
                      for h in range(4):
                          blk, r0 = h // 2, 64 * (h % 2)
                          P.add("pe", lambda e, ba=ba, h=h, blk=blk, r0=r0: e.matmul(ps[ba][:, h * 128:(h + 1) * 128],
                                                                                     lhsT=kA[:, blk, :], rhs=qAz[h % 2][:, blk, :],
                                                                                     start=True, stop=True),
                                r=["kA", ("qA", h % 2)], w=[("ps", ba)])
                      P.add("dve", lambda e, ba=ba: e.tensor_tensor(out=attT2[tt % 2][:], in0=ps[ba][:, :].rearrange("p (h t) -> p h t", t=128),
                                                                    in1=A(cb, 128, [[288, 128], [0, 4], [1, 128]]), op=ALU.mult),
                            r=[("ps", ba), "cb"], w=[("attT", tt % 2)])
                      yield
                  def gla_back(tt):
                      csl = slice(tt * 128, (tt + 1) * 128)
                      bo = nbank(ringB)
                      for h in range(4):
                          blk, r0 = h // 2, 64 * (h % 2)
                          P.add("pe", lambda e, bo=bo, h=h, tt=tt: e.matmul(ps[bo][:, h * 128:(h + 1) * 128], lhsT=attT2[tt % 2][:, h, :],
                                                                            rhs=gv[:, tt, h * 128:(h + 1) * 128], start=True, stop=False),
                                r=[("attT", tt % 2), ("gv", tt)], w=[("ps", bo)])
                          P.add("pe", lambda e, bo=bo, h=h, blk=blk, r0=r0: e.matmul(ps[bo][:, h * 128:(h + 1) * 128],
                                                                                     lhsT=qBz2[tt % 2][h % 2][:, blk, :], rhs=Sb[:, blk, :],
                                                                                     start=False, stop=True),
                                r=[("qB", tt % 2, h % 2), "Sb"], w=[("ps", bo)])
                      yield
                      bkv = nbank(ringB)
                      for h in range(4):
                          blk, r0 = h // 2, 64 * (h % 2)
                          P.add("pe", lambda e, bkv=bkv, h=h, blk=blk, r0=r0, tt=tt: e.matmul(
                              ps[bkv][r0:r0 + 64, blk * 128:(blk + 1) * 128], lhsT=kC2[tt % 2][:, h * 64:(h + 1) * 64],
                              rhs=gv[:, tt, h * 128:(h + 1) * 128], start=True, stop=True),
                              r=[("kC", tt % 2), ("gv", tt)], w=[("ps", bkv)])
                      for blk in range(2):
                          P.add("dve", lambda e, bkv=bkv, blk=blk: e.scalar_tensor_tensor(
                              out=Sf[:, blk, :], in0=Sf[:, blk, :], scalar=dec2[tt % 2][:, blk:blk + 1], in1=ps[bkv][:, blk * 128:(blk + 1) * 128],
                              op0=ALU.mult, op1=ALU.add), r=[("ps", bkv), ("dec", tt % 2), "Sf"], w=["Sf"])
                      P.add("act", lambda e: e.activation(out=Sb[:], in_=Sf[:], func=AF.Copy), r=["Sf"], w=["Sb"])
                      yield
                      for h in range(4):
                          P.add("act", lambda e, bo=bo, h=h: e.activation(out=junk[:, 0:128], in_=ps[bo][:, h * 128:(h + 1) * 128], func=AF.Square,
                                                                         scale=float(128.0 ** -0.5), accum_out=oms[:, h:h + 1]),
                                r=[("ps", bo)], w=JUNK_RES + ["oms"])
                      P.add("act", lambda e: e.activation(out=orst[:], in_=oms[:], func=AF.Ln, bias=EPS, scale=1.0),
                            r=["oms"], w=["orst"])
                      P.add("act", lambda e: e.activation(out=orst[:], in_=orst[:], func=AF.Exp, scale=-0.5), r=["orst"], w=["orst"])
                      for h in range(4):
                          P.add("dve", lambda e, bo=bo, h=h, tt=tt: e.scalar_tensor_tensor(
                              out=on_[:, h * 128:(h + 1) * 128], in0=ps[bo][:, h * 128:(h + 1) * 128], scalar=orst[:, h:h + 1],
                              in1=t3[:, tt, h * 128:(h + 1) * 128], op0=ALU.mult, op1=ALU.mult),
                              r=[("ps", bo), "orst", ("t3", tt)], w=["on"])
                      yield
                      btr = nbank(ringB)
                      for h in range(4):
                          P.add("pe", lambda e, btr=btr, h=h: e.matmul(ps[btr][:, h * 128:(h + 1) * 128], lhsT=on_[:, h * 128:(h + 1) * 128],
                                                                       rhs=ident_b, start=True, stop=True),
                                r=["on", "cb"], w=[("ps", btr)])
                      P.add("act", lambda e, btr=btr, csl=csl: e.activation(out=attnT[:, 0:4, csl],
                                                                           in_=ps[btr][:, :].rearrange("p (h t) -> p h t", t=128), func=AF.Copy),
                            r=[("ps", btr)], w=[("attnT_g", tt)])
                      yield
                  yield from gla_front(0)
                  for tt_ in range(NT):
                      gens = [gla_back(tt_)] + ([gla_front(tt_ + 1)] if tt_ + 1 < NT else [])
                      while gens:
                          for g_ in list(gens):
                              try:
                                  next(g_)
                              except StopIteration:
                                  gens.remove(g_)
                          yield
              def moba_gen():
                  P.add("dve", lambda e, T=T: e.tensor_reduce(out=kmsum[:, :, 2 * T:2 * T + 2],
                                                         in_=Kt[:, :, T * TB:(T + 1) * TB].rearrange("p a (j s) -> p a j s", s=256),
                                                         axis=AX.X, op=ALU.add),
                        r=[("Kt", pr, T) for pr in range(4)], w=["kmsum"])
                  P.add("act", lambda e: e.activation(out=kmean[:], in_=kmsum[:], func=AF.Identity, scale=1.0 / 256.0), r=["kmsum"], w=["kmean"])
                  for (src, dst, nm) in ((Qt[:, :, :], qn2, "qn2"), (Kt[:, :, T * TB:(T + 1) * TB], kn2, "kn2")):
                      rr = [("Qt", pr) for pr in range(4)] if nm == "qn2" else [("Kt", pr, T) for pr in range(4)]
                      P.add("act", lambda e, src=src: e.activation(out=sq, in_=src, func=AF.Square), r=rr, w=SQ_RES)
                      bn = nbank(ringM)
                      for pr in range(4):
                          P.add("pe", lambda e, bn=bn, pr=pr: e.matmul(ps[bn][0:8, :], lhsT=cb[:, 256 + pr * 8:264 + pr * 8],
                                                                       rhs=sq[:, pr, :], start=(pr == 0), stop=(pr == 3)),
                                r=SQ_RES + ["cb"], w=[("ps", bn)])
                      P.add("dve", lambda e, bn=bn, dst=dst: e.tensor_reduce(out=dst[:], in_=ps[bn][0:8, :], axis=AX.X, op=ALU.max),
                            r=[("ps", bn)], w=[nm])
                  P.add("dve", lambda e: e.tensor_tensor(out=kn2r[:], in0=kn2r[:], in1=kn2[:], op=ALU.max), r=["kn2", "kn2r"], w=["kn2r"])
                  P.add("dve", lambda e: e.tensor_tensor(out=bnd[:], in0=qn2[:], in1=kn2r[:], op=ALU.mult), r=["qn2", "kn2r"], w=["bnd"])
                  P.add("act", lambda e: e.activation(out=bnd[:], in_=bnd[:], func=AF.Ln, bias=EPS, scale=1.0), r=["bnd"], w=["bnd"])
                  P.add("act", lambda e: e.activation(out=bnd[:], in_=bnd[:], func=AF.Exp, scale=0.5), r=["bnd"], w=["bnd"])
                  P.add("dve", lambda e: e.tensor_scalar(out=bnd8[:], in0=cf[0:8, 1024:1032], scalar1=bnd[:, 0:1], scalar2=None, op0=ALU.mult),
                        r=["bnd", "cf"], w=["bnd8"])
                  bb = nbank(ringM)
                  P.add("pe", lambda e, bb=bb: e.matmul(ps[bb][:, 0:8], lhsT=cf[0:8, 128:256], rhs=bnd8[:], start=True, stop=True),
                        r=["bnd8", "cf"], w=[("ps", bb)])
                  P.add("dve", lambda e, bb=bb: e.tensor_scalar(out=nbias[:], in0=ps[bb][:, 0:8], scalar1=-1.03, scalar2=-0.5,
                                                                op0=ALU.mult, op1=ALU.add), r=[("ps", bb)], w=["nbias"])
                  P.add("dve", lambda e: e.tensor_tensor(out=nbias[:], in0=nbias[:], in1=b31bc[:], op=ALU.add), r=["nbias", "b31bc"], w=["nbias"])

                  need_mask = (T >= 2)
                  if need_mask:
                      for tt in range(NT):
                          own = 2 * T + tt // 2
                          bgs = [nbank(ringM), nbank(ringM)]
                          for h in range(8):
                              pr, r0 = h // 2, 64 * (h % 2)
                              bg = bgs[h % 2]
                              P.add("pe", lambda e, bg=bg, h=h, pr=pr, r0=r0, tt=tt: e.matmul(
                                  ps[bg][:, pr * 8:(pr + 1) * 8], lhsT=Qt[r0:r0 + 64, pr, tt * 128:(tt + 1) * 128],
                                  rhs=kmean[r0:r0 + 64, pr, :], start=True, stop=True),
                                  r=[("Qt", pr), "kmean"], w=[("ps", bg)])
                          for par in range(2):
                              P.add("dve", lambda e, bg=bgs[par], own=own, par=par: e.tensor_tensor(
                                  out=A(gmx, par * 8, [[64, 128], [16, 4], [1, 8]]),
                                  in0=ps[bg][:, 0:32].rearrange("p (a n) -> p a n", n=8),
                                  in1=cf[:, 512 + own * 64:512 + own * 64 + 32].rearrange("p (a n) -> p a n", n=8), op=ALU.add),
                                  r=[("ps", bgs[par]), "cf"], w=["gmx"])
                          for h in range(8):
                              P.add("dve", lambda e, h=h: e.max(out=m8[:, h, :], in_=gmx[:, h * 8:(h + 1) * 8]), r=["gmx"], w=["m8"])
                          P.add("dve", lambda e: e.tensor_tensor(out=selt[:].rearrange("p (h n) -> p h n", n=8),
                                                                 in0=gmx[:].rearrange("p (h n) -> p h n", n=8),
                                                                 in1=A(m8, 3, [[64, 128], [8, 8], [0, 8]]), op=ALU.is_ge),
                                r=["gmx", "m8"], w=["selt"])
                          P.add("dve", lambda e, tt=tt: e.tensor_scalar(out=mrow[:, tt, :], in0=selt[:], scalar1=-1.0, scalar2=BIG,
                                                                        op0=ALU.add, op1=ALU.mult), r=["selt"], w=[("mrow", tt)])

                  LOOK = 1
                  tl = []
                  for j in range(i1 + 1):
                      for c in range(2):
                          if j < i0:
                              lo = 0
                          elif j == i0:
                              lo = 128 * c
                          else:
                              lo = 256 + 128 * c
                          tl.append((j, c, lo))
                  items = [(pr, ti, j, c, lo) for pr in range(4) for ti, (j, c, lo) in enumerate(tl)]
                  ntl = len(tl)

                  def mask_prep(pr):
                      bm = nbank(ringM)
                      for par in range(2):
                          h = 2 * pr + par
                          r0 = 64 * par
                          for tt in range(NT):
                              P.add("pe", lambda e, bm=bm, tt=tt, h=h, r0=r0: e.matmul(ps[bm][r0:r0 + 8, tt * 128:(tt + 1) * 128],
                                                                                       lhsT=mrow[:, tt, h * 8:(h + 1) * 8], rhs=ident_b,
                                                                                       start=True, stop=True),
                                    r=[("mrow", tt), "cb"], w=[("ps", bm)])
                      mT = maskT[pr % 2]
                      for par in range(2):
                          r0 = 64 * par
                          P.add("act", lambda e, bm=bm, mT=mT, r0=r0: e.activation(out=mT[r0:r0 + 8, :], in_=ps[bm][r0:r0 + 8, :], func=AF.Copy),
                                r=[("ps", bm)], w=[("maskT", pr % 2, par)])

                  if need_mask:
                      mask_prep(0)
                  yield
                  for idx in range(len(items) + LOOK):
                      if idx < len(items):
                          pr, ti, j, c, lo = items[idx]
                          mT = maskT[pr % 2]
                          if need_mask and ti == 0 and pr + 1 < 4:
                              mask_prep(pr + 1)
                          kt = 2 * j + c
                          use_mask = need_mask and (j < i1)
                          bss = [nbank(ringM), nbank(ringM)]
                          for par in range(2):
                              r0 = 64 * par
                              P.add("pe", lambda e, bs_=bss[par], kt=kt, lo=lo, pr=pr, r0=r0, use_mask=use_mask: e.matmul(
                                  ps[bs_][:, lo:TB], lhsT=Kt[r0:r0 + 64, pr, kt * 128:(kt + 1) * 128], rhs=Qt[r0:r0 + 64, pr, lo:TB],
                                  start=True, stop=(not use_mask)),
                                  r=[("Kt", pr, kt // 4), ("Qt", pr)], w=[("ps", bss[par])])
                          if use_mask:
                              for par in range(2):
                                  r0 = 64 * par
                                  P.add("pe", lambda e, bs_=bss[par], j=j, lo=lo, mT=mT, r0=r0: e.matmul(
                                      ps[bs_][:, lo:TB], lhsT=ohk[r0:r0 + 8, j * 128:(j + 1) * 128], rhs=mT[r0:r0 + 8, lo:TB], start=False, stop=True),
                                      r=["ohk", "ohk2", ("maskT", pr % 2, par)], w=[("ps", bss[par])])
                          for par in range(2):
                              h = 2 * pr + par
                              pi = (2 * idx + par) % NPT
                              pt = Pt[pi]
                              pres = ("Pt", pi)
                              P.add("act", lambda e, bs_=bss[par], lo=lo, pt=pt, h=h: e.activation(out=pt[:, lo:TB], in_=ps[bs_][:, lo:TB], func=AF.Exp,
                                                                                                 bias=nbias[:, h:h + 1], scale=1.0),
                                    r=[("ps", bss[par]), "nbias"], w=[pres])
                              fix = []
                              if j == i0:
                                  fix.append((lo, 256, Mown[:, h, 0:256 - lo]))
                                  if c == 1:
                                      fix.append((256, 368, Mprev[:, h, :]))
                              elif j == i1:
                                  fix.append((lo, 512, Mown[:, h, 0:512 - lo]))
                              elif j == i0 - 1 and c == 1:
                                  fix.append((0, 112, Mprev[:, h, :]))
                              for (a0, a1, tab) in fix:
                                  P.add("dve", lambda e, pt=pt, a0=a0, a1=a1, tab=tab: e.tensor_tensor(out=pt[:, a0:a1], in0=pt[:, a0:a1], in1=tab,
                                                                                                      op=ALU.mult),
                                        r=[pres, ("Mown", h), ("Mprev", h)], w=[pres])
                      if idx - LOOK >= 0:
                          pr, ti, j, c, lo = items[idx - LOOK]
                          kt = 2 * j + c
                          for par in range(2):
                              h = 2 * pr + par
                              r0 = 64 * par
                              dr0 = 64 - r0
                              acc = 6 + par
                              pi = (2 * (idx - LOOK) + par) % NPT
                              pt = Pt[pi]
                              pres = ("Pt", pi)
                              vcol = pr * 192 + (0 if par == 0 else 64)
                              P.add("pe", lambda e, acc=acc, kt=kt, lo=lo, pt=pt, vcol=vcol, ti=ti, ntl=ntl: e.matmul(
                                  ps[acc][:, lo:TB], lhsT=Vint[:, kt, vcol:vcol + 128], rhs=pt[:, lo:TB],
                                  start=(ti == 0), stop=(ti == ntl - 1), skip_group_check=True),
                                  r=[pres, ("V", kt)], w=[("ps", acc)])
                              if ti == ntl - 1:
                                  rd = rden2[par]
                                  rres = ("rden", par)
                                  P.add("dve", lambda e, acc=acc, r0=r0, dr0=dr0, rd=rd: e.reciprocal(out=rd[r0:r0 + 64, :], in_=ps[acc][dr0:dr0 + 64, :]),
                                        r=[("ps", acc)], w=[rres])
                                  P.add("dve", lambda e, acc=acc, r0=r0, pr=pr, rd=rd: e.tensor_tensor(out=attnT[r0:r0 + 64, 4 + pr, :], in0=ps[acc][r0:r0 + 64, :],
                                                                                                in1=rd[r0:r0 + 64, :], op=ALU.mult),
                                        r=[("ps", acc), rres], w=[("attnT_m", h)])
                      yield
              ringG.banks, ringB.banks, ringM.banks = [0], [1, 2], [3, 4, 5]
              ringG.reset()
              ringB.reset()
              ringM.reset()
              gg_ = gla_gen()
              mg_ = moba_gen()
              ratio = max(1, int(round(8.0 * (i1 + 1) / 36.0)))
              alive_g, alive_m = True, True
              while alive_g or alive_m:
                  if alive_g:
                      try:
                          next(gg_)
                      except StopIteration:
                          alive_g = False
                  for _ in range(ratio if alive_g else 4):
                      if alive_m:
                          try:
                              next(mg_)
                          except StopIteration:
                              alive_m = False
              if isdbg:
                  dump("attnT_g", attnT[:, 0:4, :], [("attnT_g", t_) for t_ in range(NT)])
              if isdbg:
                  dump("attnT_m", attnT[:, 4:8, :], [("attnT_m", h_) for h_ in range(8)])
              if stop_after == "moba":
                  break

              ATT_ALL = [("attnT_g", tt) for tt in range(NT)] + [("attnT_m", h) for h in range(8)]
              wvs = [wload(wb_out_v[:, :, ch * 512:(ch + 1) * 512], "wb_out", 8, 512) for ch in range(2)]
              for tt in range(NT):
                  for ch in range(2):
                      wv, wr = wvs[ch]
                      bk = nbank()
                      acc_mm(ps[bk][:, :], [(attnT[:, kc, tt * 128:(tt + 1) * 128], wv[:, kc, :]) for kc in range(8)],
                             r=[wr] + ATT_ALL, w=[("ps", bk)])
                      P.add("dve", lambda e, bk=bk, ch=ch: e.tensor_tensor(out=sig[:], in0=ps[bk][:, :], in1=gate_a[:, ch * 512:(ch + 1) * 512],
                                                                           op=ALU.mult), r=[("ps", bk), ("gate_a", ch)], w=["sig"])
                      P.add("dve", lambda e, tt=tt, ch=ch: e.tensor_tensor(out=xblk[:, tt, ch * 512:(ch + 1) * 512],
                                                                           in0=xblk[:, tt, ch * 512:(ch + 1) * 512], in1=sig[:], op=ALU.add),
                            r=["sig", ("x", tt)], w=[("x", tt)])
              if isdbg:
                  dump("x1", xblk[:].rearrange("p t d -> p (t d)"), [("x", t_) for t_ in range(NT)])
              norm_hT(b, 1)
              for pc in range(8):
                  wv, wr = wload(wb_1_v[:, :, pc * 512:(pc + 1) * 512], "wb_1", 8, 512)
                  for fl in range(4):
                      fc = pc * 4 + fl
                      bk = nbank()
                      acc_mm(ps[bk][:, :], [(wv[:, kc, fl * 128:(fl + 1) * 128], hT[:, kc, :]) for kc in range(8)],
                             r=[wr] + HT_ALL, w=[("ps", bk)])
                      P.add("act", lambda e, bk=bk, fc=fc: e.activation(out=uT[:, fc, :], in_=ps[bk][:, :], func=AF.Relu),
                            r=[("ps", bk)], w=[("uT", fc)])
                      P.add("dve", lambda e, fc=fc: e.tensor_tensor(out=uT[:, fc, :], in0=uT[:, fc, :], in1=uT[:, fc, :], op=ALU.mult),
                            r=[("uT", fc)], w=[("uT", fc)])
              for pc in range(8):
                  wv, wr = wload(wb_2_v[:, pc * 4:(pc + 1) * 4, :], "wb_2", 4, 1024)
                  for fl in range(4):
                      fc = pc * 4 + fl
                      for tt in range(NT):
                          for ch in range(2):
                              bk = tt * 2 + ch
                              P.add("pe", lambda e, bk=bk, fc=fc, fl=fl, tt=tt, ch=ch, wv=wv: e.matmul(
                                  ps[bk][:, :], lhsT=uT[:, fc, tt * 128:(tt + 1) * 128], rhs=wv[:, fl, ch * 512:(ch + 1) * 512],
                                  start=(fc == 0), stop=(fc == 31), skip_group_check=True),
                                  r=[wr, ("uT", fc)], w=[("ps", bk)])
              for tt in range(NT):
                  for ch in range(2):
                      bk = tt * 2 + ch
                      P.add("dve", lambda e, bk=bk, ch=ch: e.tensor_tensor(out=sig[:], in0=ps[bk][:, :], in1=gate_m[:, ch * 512:(ch + 1) * 512],
                                                                           op=ALU.mult), r=[("ps", bk), ("gate_m", ch)], w=["sig"])
                      P.add("dve", lambda e, tt=tt, ch=ch: e.tensor_tensor(out=ob[tt % 2][:, ch * 512:(ch + 1) * 512],
                                                                           in0=xblk[:, tt, ch * 512:(ch + 1) * 512], in1=sig[:], op=ALU.add),
                            r=["sig", ("x", tt)], w=[("ob", tt % 2, ch)])
                  obr = [("ob", tt % 2, 0), ("ob", tt % 2, 1)]
                  P.add("act", lambda e, tt=tt: e.activation(out=junk, in_=ob[tt % 2][:], func=AF.Square, scale=1.0 / 32.0,
                                                             accum_out=ms[:, tt:tt + 1]),
                        r=obr, w=JUNK_RES + [("ms", tt)])
                  P.add("act", lambda e, tt=tt: e.activation(out=rstd[:, tt:tt + 1], in_=ms[:, tt:tt + 1], func=AF.Ln, bias=EPS, scale=1.0),
                        r=[("ms", tt)], w=[("rstd", tt)])
                  P.add("act", lambda e, tt=tt: e.activation(out=rstd[:, tt:tt + 1], in_=rstd[:, tt:tt + 1], func=AF.Exp, scale=-0.5),
                        r=[("rstd", tt)], w=[("rstd", tt)])
                  P.add("dve", lambda e, tt=tt: e.scalar_tensor_tensor(out=ob[tt % 2][:], in0=ob[tt % 2][:], scalar=rstd[:, tt:tt + 1],
                                                                       in1=gfin_bc[:], op0=ALU.mult, op1=ALU.mult),
                        r=obr + [("rstd", tt), "gfin_bc"], w=obr)
                  odst = out_d.ap()[b, T * TB + tt * 128:T * TB + (tt + 1) * 128, :]
                  op = P.add("sp", lambda e, odst=odst, tt=tt: e.dma_start(out=odst, in_=ob[tt % 2][:]), r=obr,
                             w=[("out", b, T, tt)], dma_key=("ostore", tt % 2))
                  out_ops.append(op)
          if stop_after is not None:
              break
    except _Stop:
        pass
    if not out_ops:
        op = P.add("sp", lambda e: e.dma_start(out=out_d.ap()[0, 0:TB, :].rearrange("(t p) d -> p t d", p=128), in_=xblk[:]),
                   r=[("x", t_) for t_ in range(NT)], w=[("out", 0, 0)], dma_key="ostore")
        out_ops.append(op)
    fin = [out_ops[-1]]
    for o in P.ops:
        if o.dma_key is not None and isinstance(o.dma_key, tuple) and o.dma_key[0] == "dbg":
            fin.append(o)
    P.finals = fin
    return nc, P


_CACHE = {}


def make_inputs_common(inputs):
    cs = host_consts()
    com = {
        "w_ada": np.ascontiguousarray(inputs["w_ada"][0], dtype=np.float32),
        "b_ada": np.ascontiguousarray(inputs["b_ada"][0].reshape(48, 128), dtype=np.float32),
        "g_mix": np.ascontiguousarray(inputs["g_mix"][0].reshape(8, 128), dtype=np.float32),
        "g_mlp": np.ascontiguousarray(inputs["g_mlp"][0].reshape(8, 128), dtype=np.float32),
        "w_in": np.ascontiguousarray(inputs["w_in"][0], dtype=np.float32),
        "w_gla_gate": np.ascontiguousarray(inputs["w_gla_gate"][0], dtype=np.float32),
        "b_gla_gate": np.ascontiguousarray(inputs["b_gla_gate"][0].reshape(1, 256), dtype=np.float32),
        "g_gla_out": np.ascontiguousarray(inputs["g_gla_out"][0].reshape(1, 512), dtype=np.float32),
        "rel_bias": np.ascontiguousarray(inputs["rel_bias"], dtype=np.float32),
        "w_out": np.ascontiguousarray(inputs["w_out"][0], dtype=np.float32),
        "w_ff1": np.ascontiguousarray(inputs["w_ff1"][0], dtype=np.float32),
        "w_ff2": np.ascontiguousarray(inputs["w_ff2"][0], dtype=np.float32),
        "g_final": np.ascontiguousarray(inputs["g_final"].reshape(1, D), dtype=np.float32),
    }
    com.update(cs)
    return com


def kernel(**inputs):
    x = np.asarray(inputs["x"], dtype=np.float32)
    c = np.asarray(inputs["c"], dtype=np.float32)
    com = make_inputs_common(inputs)
    nc, P = build_nc(SEQ_PER_CORE, NBLK)
    P.emit(nc)
    in_maps = []
    for k in range(NCORES):
        m = dict(com)
        m["x"] = np.ascontiguousarray(x[k * SEQ_PER_CORE:(k + 1) * SEQ_PER_CORE])
        m["c"] = np.ascontiguousarray(c[k * SEQ_PER_CORE:(k + 1) * SEQ_PER_CORE])
        in_maps.append(m)
    res = run_bass_kernel_spmd(nc, in_maps, core_ids=list(range(NCORES)))
    out = np.concatenate([np.asarray(r["out"], dtype=np.float32) for r in res.results], axis=0)
    return out
```

```python
import numpy as np
import concourse.bass as bass
import concourse.mybir as mybir
from concourse.bass_utils import run_bass_kernel_spmd

F32 = mybir.dt.float32
BF16 = mybir.dt.bfloat16
AF = mybir.ActivationFunctionType
ALU = mybir.AluOpType
AX = mybir.AxisListType

D = 1024
S = 2048
NCORES = 8
SEQ_PER_CORE = 4
TB = 512
NT = 4
NBLK = S // TB
DIN = 3088
DFF = 4096
BIG = 30000.0
GBIG = 10000.0
EPS = 1e-6
LT = 768
SAME_ENGINE_SYNC = True


class Op:
    __slots__ = ("idx", "eng", "fn", "deps", "dma_key", "ticket", "signal", "pos")

    def __init__(self, idx, eng, fn, dma_key):
        self.idx = idx
        self.eng = eng
        self.fn = fn
        self.deps = set()
        self.dma_key = dma_key
        self.ticket = None
        self.signal = dma_key is not None
        self.pos = None


class Prog:
    ENGS = ("pe", "act", "dve", "pool", "sp")

    def __init__(self):
        self.ops = []
        self.lastw = {}
        self.rd = {}
        self.finals = []

    def add(self, eng, fn, r=(), w=(), dma_key=None):
        op = Op(len(self.ops), eng, fn, dma_key)
        deps = op.deps
        for res in r:
            o = self.lastw.get(res)
            if o is not None:
                deps.add(o)
            if isinstance(res, tuple) and res[0] == "ps":
                for o in self.rd.get(res, ()):
                    if o.eng != eng:
                        deps.add(o)
        for res in w:
            o = self.lastw.get(res)
            if o is not None:
                deps.add(o)
            for o in self.rd.get(res, ()):
                deps.add(o)
        for res in r:
            self.rd.setdefault(res, []).append(op)
        for res in w:
            self.lastw[res] = op
            self.rd[res] = []
        deps.discard(op)
        self.ops.append(op)
        return op

    def emit(self, nc):
        for op in self.ops:
            for d in op.deps:
                if d.dma_key is None and (d.eng != op.eng or (SAME_ENGINE_SYNC and op.eng != "pe")):
                    d.signal = True
        for op in self.finals:
            op.signal = True
        cnt = {e: 0 for e in self.ENGS}
        dcnt = {}
        for op in self.ops:
            if op.dma_key is not None:
                dcnt[op.dma_key] = dcnt.get(op.dma_key, 0) + 16
                op.ticket = dcnt[op.dma_key]
            elif op.signal:
                cnt[op.eng] += 1
                op.ticket = cnt[op.eng]
        keys = sorted(dcnt.keys(), key=str)
        import contextlib
        with contextlib.ExitStack() as es:
            esem = {e: es.enter_context(nc.semaphore("e_" + e)) for e in self.ENGS}
            dsem = {k: es.enter_context(nc.semaphore("d_" + str(i))) for i, k in enumerate(keys)}
            block = es.enter_context(nc.Block())
            by_eng = {e: [op for op in self.ops if op.eng == e] for e in self.ENGS}

            def run(engname, e):
                known = {}
                for op in by_eng[engname]:
                    waits = {}
                    for d in op.deps:
                        if d.dma_key is not None:
                            sem = dsem[d.dma_key]
                        else:
                            if d.eng == op.eng and (not SAME_ENGINE_SYNC or op.eng == "pe"):
                                continue
                            sem = esem[d.eng]
                        k = id(sem)
                        if d.ticket > waits.get(k, (None, 0))[1]:
                            waits[k] = (sem, d.ticket)
                    for k, (sem, v) in waits.items():
                        if known.get(k, 0) >= v:
                            continue
                        known[k] = v
                        e.wait_ge(sem, v)
                    ins = op.fn(e)
                    if op.dma_key is not None:
                        ins.then_inc(dsem[op.dma_key], 16)
                    elif op.signal:
                        ins.then_inc(esem[op.eng], 1)
                if engname == "sp":
                    for k in keys:
                        e.wait_ge(dsem[k], dcnt[k])

            @block.tensor
            def _(e):
                run("pe", e)

            @block.scalar
            def _(e):
                run("act", e)

            @block.vector
            def _(e):
                run("dve", e)

            @block.gpsimd
            def _(e):
                run("pool", e)

            @block.sync
            def _(e):
                run("sp", e)


def _t5_bucket(rel):
    n = np.maximum(rel, 0)
    max_exact = 16
    ratio = np.maximum(n, 1).astype(np.float32) / np.float32(max_exact)
    large = max_exact + (np.log(ratio).astype(np.float32) / np.float32(np.log(128 / 16)) * np.float32(16)).astype(np.int32)
    large = np.minimum(large, 31)
    return np.where(n < max_exact, n, large)


def host_consts():
    cf = np.zeros((128, 1032), np.float32)
    cf[:, 0:128] = np.eye(128)
    cf[:, 128:256] = 1.0
    j = np.arange(128)[:, None]
    i = np.arange(128)[None, :]
    cf[:, 256:384] = np.where(j <= i, -1.0 / 16, 0.0)
    cf[:, 384:512] = np.where(j > i, -1.0 / 16, 0.0)
    gm = np.zeros((8, 8, 8), np.float32)
    for own in range(8):
        for n in range(8):
            gm[own, :, n] = GBIG if n == own else (-GBIG if n > own else 0.0)
    cf[:, 512:1024] = gm.reshape(1, 512)
    for h in range(8):
        cf[h, 1024 + h] = 1.0
    cb = np.zeros((128, 288), np.float32)
    cb[:, 0:128] = np.eye(128)
    cb[:, 128:256] = np.where(j <= i, 1.0, 0.0)
    for pr in range(4):
        cb[0:64, 256 + pr * 8 + 2 * pr] = 1.0
        cb[64:128, 256 + pr * 8 + 2 * pr + 1] = 1.0
    ohk = np.zeros((8, 8 * 128), np.float32)
    for jj in range(8):
        ohk[jj, jj * 128:(jj + 1) * 128] = 1.0
    ehot = np.zeros((33, LT), np.float32)
    u = np.arange(LT)
    rel = u - 255
    bk = _t5_bucket(rel)
    for uu in range(LT):
        if rel[uu] >= 0:
            ehot[bk[uu], uu] += 1.0
        else:
            ehot[32, uu] = -BIG
    ehot[31, :] -= 1.0
    return {"cf": cf, "cb": cb, "ohk": ohk, "ehot": ehot}


def build_nc(nseq=SEQ_PER_CORE, nblk=NBLK, dbg=None, stop_after=None):
    nc = bass.Bass("TRN2", target_bir_lowering=False)
    P = Prog()
    dbg = dbg or {}

    def din(name, shape):
        return nc.dram_tensor(name, list(shape), F32, kind="ExternalInput")

    x_d = din("x", (nseq, S, D))
    c_d = din("c", (4, D))
    w_ada_d = din("w_ada", (D, 6 * D))
    b_ada_d = din("b_ada", (48, 128))
    g_mix_d = din("g_mix", (8, 128))
    g_mlp_d = din("g_mlp", (8, 128))
    w_in_d = din("w_in", (D, DIN))
    w_gg_d = din("w_gla_gate", (16, 256))
    b_gg_d = din("b_gla_gate", (1, 256))
    g_out_d = din("g_gla_out", (1, 512))
    relb_d = din("rel_bias", (32, 8))
    w_out_d = din("w_out", (D, D))
    w_ff1_d = din("w_ff1", (D, DFF))
    w_ff2_d = din("w_ff2", (DFF, D))
    g_fin_d = din("g_final", (1, D))
    cf_d = din("cf", (128, 1032))
    cb_d = din("cb", (128, 288))
    ohk_d = din("ohk", (8, 1024))
    ehot_d = din("ehot", (33, LT))
    out_d = nc.dram_tensor("out", [nseq, S, D], F32, kind="ExternalOutput")
    dbg_t = {k: nc.dram_tensor("dbg_" + k, list(shp), F32, kind="ExternalOutput") for k, shp in dbg.items()}

    wb_in = nc.dram_tensor("wb_in", [D, DIN], BF16)
    wb_out = nc.dram_tensor("wb_out", [D, D], BF16)
    wb_1 = nc.dram_tensor("wb_1", [D, DFF], BF16)
    wb_2 = nc.dram_tensor("wb_2", [DFF, D], BF16)
    TOEPN = 128 * LT + 1024
    toep = nc.dram_tensor("toep", [8 * TOEPN], F32)

    def sb(name, shape, dt):
        return nc.alloc_sbuf_tensor("s_" + name, list(shape), dt)

    def A(t, off, dims):
        return bass.AP(t, off, [list(d) for d in dims])

    cf = sb("cf", (128, 1032), F32)
    cb = sb("cb", (128, 288), BF16)
    ohk = sb("ohk", (128, 1024), BF16)
    wslot = [sb("wslot%d" % i, (128, 4096), BF16) for i in range(2)]
    xblk = sb("xblk", (128, NT, D), F32)
    hT = sb("hT", (128, 8, TB), BF16)
    gqT = sb("gqT", (128, 2, TB), F32)
    gkT = sb("gkT", (128, 2, TB), F32)
    gktok = sb("gktok", (128, NT, 256), F32)
    gv = sb("gv", (128, NT, 512), BF16)
    t3 = sb("t3", (128, NT, 512), BF16)
    sig = sb("sig", (128, 512), F32)
    ggT = sb("ggT", (17, TB), BF16)
    wg = sb("wg", (17, 256), BF16)
    Qt = sb("Qt", (128, 4, TB), BF16)
    Kt = sb("Kt", (128, 4, S), BF16)
    Vint = sb("Vint", (128, 16, 768), BF16)
    maskT = [sb("maskT%d" % i, (128, TB), BF16) for i in range(2)]
    attnT = sb("attnT", (128, 8, TB), BF16)
    arena = sb("arena", (128, 8192), F32)
    gate_a = sb("gate_a", (128, D), F32)
    gate_m = sb("gate_m", (128, D), F32)
    gout_bc = sb("gout_bc", (128, 512), F32)
    gfin_bc = sb("gfin_bc", (128, D), F32)
    Mown = sb("Mown", (128, 8, 256), BF16)
    Mprev = sb("Mprev", (128, 8, 112), BF16)
    b31bc = sb("b31bc", (128, 8), F32)
    Lg = sb("Lg", (128, 256), F32)
    eA = sb("eA", (128, 2, 128), F32)
    eK = sb("eK", (128, 2, 128), F32)
    eB = sb("eB", (128, 2, 128), F32)
    eC = sb("eC", (128, 256), F32)
    qAz = [sb("qAz%d" % i, (128, 2, 128), BF16) for i in range(2)]
    kA = sb("kA", (128, 2, 128), BF16)
    qBz2 = [[sb("qBz%d_%d" % (j, i), (128, 2, 128), BF16) for i in range(2)] for j in range(2)]
    kC2 = [sb("kC%d" % i, (128, 256), BF16) for i in range(2)]
    attT2 = [sb("attT%d" % i, (128, 4, 128), BF16) for i in range(2)]
    on_ = sb("on", (128, 512), BF16)
    Sf = sb("Sf", (128, 2, 128), F32)
    Sb = sb("Sb", (128, 2, 128), BF16)
    brf = sb("brf", (128, 2), F32)
    nbrf = sb("nbrf", (128, 2), F32)
    dec2 = [sb("dec%d" % i, (128, 2), F32) for i in range(2)]
    oms = sb("oms", (128, 4), F32)
    orst = sb("orst", (128, 4), F32)
    ms = sb("ms", (128, NT), F32)
    rstd = sb("rstd", (128, NT), F32)
    NPT = 6
    Pt = [sb("Pt%d" % i, (128, TB), BF16) for i in range(NPT)]
    gmx = sb("gmx", (128, 64), F32)
    m8 = sb("m8", (128, 8, 8), F32)
    selt = sb("selt", (128, 64), F32)
    mrow = sb("mrow", (128, NT, 64), BF16)
    kmsum = sb("kmsum", (128, 4, 8), F32)
    kmean = sb("kmean", (128, 4, 8), BF16)
    qn2 = sb("qn2", (8, 1), F32)
    kn2 = sb("kn2", (8, 1), F32)
    kn2r = sb("kn2r", (8, 1), F32)
    bnd = sb("bnd", (8, 1), F32)
    bnd8 = sb("bnd8", (8, 8), F32)
    nbias = sb("nbias", (128, 8), F32)
    cactT = sb("cactT", (128, 32), F32)
    b48 = sb("b48", (48, 128), F32)
    g8 = sb("g8", (8, 2, 128), F32)
    badaT = sb("badaT", (128, 48), F32)
    gT = sb("gT", (128, 2, 8), F32)
    adaT = sb("adaT", (128, 48, 4), F32)
    G1T = sb("G1T", (128, 8, 4), F32)
    G2T = sb("G2T", (128, 8, 4), F32)
    diag = [sb("diag%d" % i, (128, 128), F32) for i in range(2)]
    rb33 = sb("rb33", (33, 8), F32)
    rbx = sb("rbx", (33, 128), F32)

    ab = arena[:].bitcast(BF16)
    uT = ab.rearrange("p (c t) -> p c t", t=TB)
    sq = ab[:, 0:4 * TB].rearrange("p (a t) -> p a t", t=TB)
    xsfull = ab[:, 8 * TB:16 * TB].rearrange("p (t d) -> p t d", d=D)
    junk = ab[:, 30 * TB:32 * TB]
    wst = arena[:, 0:4096].rearrange("p (s k n) -> p s k n", s=2, k=8)
    Gtabs = [arena[:, 4096:4096 + LT], arena[:, 4864:4864 + LT]]
    ehot_sb = arena[0:33, 5632:5632 + LT]
    c4 = arena[0:4, 6400:7424]
    c4e = gate_m[0:4, :]
    rden2 = [sb("rdenA", (128, TB), F32), sb("rdenB", (128, TB), F32)]
    XS_RES = [[("uT", 8 + 2 * t_), ("uT", 9 + 2 * t_)] for t_ in range(NT)]
    JUNK_RES = [("uT", 30), ("uT", 31)]
    SQ_RES = [("uT", c_) for c_ in range(4)]

    NB_RING = 6
    ps = [nc.alloc_psum_tensor("ps%d" % i, [128, 512], F32) for i in range(8)]
    class Ring:
        def __init__(self, banks):
            self.banks = list(banks)
            self.i = 0

        def reset(self):
            self.i = 0

    ringD = Ring(range(6))
    ringG = Ring([0])
    ringB = Ring([1, 2])
    ringM = Ring([2, 3, 4, 5])

    def nbank(ring=None):
        ring = ring or ringD
        k = ring.banks[ring.i % len(ring.banks)]
        ring.i += 1
        return k

    ident_f = cf[:, 0:128]
    ones_f = cf[:, 128:256]
    ident_b = cb[:, 0:128]

    def dump(name, src_ap, res):
        if name in dbg_t:
            t = dbg_t[name]
            P.add("pool", lambda e: e.dma_start(out=t.ap(), in_=src_ap), r=res, w=[("dbg", name)], dma_key=("dbg", name))

    P.add("sp", lambda e: e.dma_start(out=cf[:], in_=cf_d.ap()[:, :]), w=["cf"], dma_key="cf")
    P.add("pool", lambda e: e.dma_start(out=cb[:], in_=cb_d.ap()[:, :]), w=["cb"], dma_key="cb")
    P.add("pool", lambda e: e.dma_start(out=ohk[0:8, :], in_=ohk_d.ap()[:, :]), w=["ohk"], dma_key="ohk")
    P.add("pool", lambda e: e.dma_start(out=ohk[64:72, :], in_=ohk_d.ap()[:, :]), w=["ohk2"], dma_key="ohk2")
    P.add("pool", lambda e: e.dma_start(out=wg[0:16, :], in_=w_gg_d.ap()[:, :]), w=["wg0"], dma_key="wg0")
    P.add("pool", lambda e: e.dma_start(out=wg[16:17, :], in_=b_gg_d.ap()[:, :]), w=["wg1"], dma_key="wg1")
    WB_RES = {}
    for (nm, dst, src, rows) in (("wb_in", wb_in, w_in_d, D), ("wb_out", wb_out, w_out_d, D),
                                 ("wb_1", wb_1, w_ff1_d, D), ("wb_2", wb_2, w_ff2_d, DFF)):
        nsp = 4
        rr = rows // nsp
        WB_RES[nm] = []
        for q in range(nsp):
            key = (nm, q)
            P.add("pool", lambda e, dst=dst, src=src, q=q, rr=rr: e.dma_start(
                out=dst.ap()[q * rr:(q + 1) * rr, :], in_=src.ap()[q * rr:(q + 1) * rr, :]),
                w=[key], dma_key=key)
            WB_RES[nm].append(key)

    P.add("sp", lambda e: e.dma_start(out=gout_bc[:], in_=A(g_out_d, 0, [[0, 128], [1, 512]])), w=["gout_bc"], dma_key="gout")
    P.add("sp", lambda e: e.dma_start(out=gfin_bc[:], in_=A(g_fin_d, 0, [[0, 128], [1, D]])), w=["gfin_bc"], dma_key="gfin")
    P.add("sp", lambda e: e.dma_start(out=b31bc[:], in_=A(relb_d, 31 * 8, [[0, 128], [1, 8]])), w=["b31bc"], dma_key="b31")
    P.add("sp", lambda e: e.dma_start(out=rb33[0:32, :], in_=relb_d.ap()[:, :]), w=["rb33a"], dma_key="rb33")
    P.add("sp", lambda e: e.dma_start(out=c4, in_=c_d.ap()[:, :]), w=["c4"], dma_key="c4")
    P.add("sp", lambda e: e.dma_start(out=b48[:], in_=b_ada_d.ap()[:, :]), w=["b48"], dma_key="b48")
    P.add("sp", lambda e: e.dma_start(out=g8[:, 0, :], in_=g_mix_d.ap()[:, :]), w=["g8a"], dma_key="g8a")
    P.add("sp", lambda e: e.dma_start(out=g8[:, 1, :], in_=g_mlp_d.ap()[:, :]), w=["g8b"], dma_key="g8b")
    P.add("sp", lambda e: e.dma_start(out=ehot_sb, in_=ehot_d.ap()[:, :]), w=["ehot"], dma_key="ehot")

    P.add("dve", lambda e: e.memset(ggT[:], 1.0), w=["ggT"])
    P.add("dve", lambda e: e.memset(Vint[:], 1.0), w=[("V", t_) for t_ in range(16)])
    P.add("dve", lambda e: e.memset(kmsum[:], 0.0), w=["kmsum"])
    for i_ in range(2):
        P.add("dve", lambda e, i_=i_: e.memset(qAz[i_][:], 0.0), w=[("qA", i_)])
        for j_ in range(2):
            P.add("dve", lambda e, i_=i_, j_=j_: e.memset(qBz2[j_][i_][:], 0.0), w=[("qB", j_, i_)])
    P.add("dve", lambda e: e.memset(kmean[:], 0.0), w=["kmean"])
    P.add("dve", lambda e: e.memset(rb33[32:33, :], 1.0), w=["rb33b"])

    P.add("act", lambda e: e.activation(out=c4e, in_=c4, func=AF.Exp, scale=-1.0), r=["c4"], w=["c4e"])
    P.add("dve", lambda e: e.tensor_scalar(out=c4e, in0=c4e, scalar1=1.0, scalar2=None, op0=ALU.add), r=["c4e"], w=["c4e"])
    P.add("dve", lambda e: e.reciprocal(out=c4e, in_=c4e), r=["c4e"], w=["c4e"])
    P.add("dve", lambda e: e.tensor_tensor(out=c4, in0=c4, in1=c4e, op=ALU.mult), r=["c4e", "c4"], w=["c4"])
    bk = nbank()
    for kc in range(8):
        P.add("pe", lambda e, kc=kc, bk=bk: e.matmul(ps[bk][:, kc * 4:kc * 4 + 4], lhsT=c4[:, kc * 128:(kc + 1) * 128],
                                                     rhs=cf[0:4, 0:4], start=True, stop=True),
              r=["c4", "cf"], w=[("ps", bk)])
    P.add("dve", lambda e, bk=bk: e.tensor_copy(out=cactT[:], in_=ps[bk][:, 0:32]), r=[("ps", bk)], w=["cactT"])
    bk = nbank()
    P.add("pe", lambda e, bk=bk: e.matmul(ps[bk][:, 0:48], lhsT=b48[0:48, :], rhs=cf[0:48, 0:48], start=True, stop=True),
          r=["b48", "cf"], w=[("ps", bk)])
    for q in range(2):
        P.add("pe", lambda e, bk=bk, q=q: e.matmul(ps[bk][:, 64 + q * 8:72 + q * 8], lhsT=g8[0:8, q, :], rhs=cf[0:8, 0:8],
                                                   start=True, stop=True),
              r=["g8a", "g8b", "cf"], w=[("ps", bk)])
    P.add("dve", lambda e, bk=bk: e.tensor_copy(out=badaT[:], in_=ps[bk][:, 0:48]), r=[("ps", bk)], w=["badaT"])
    P.add("dve", lambda e, bk=bk: e.tensor_copy(out=gT[:], in_=ps[bk][:, 64:80].rearrange("p (q c) -> p q c", c=8)),
          r=[("ps", bk)], w=["gT"])
    bk_ada = nbank()
    w_ada_v = w_ada_d.ap().rearrange("(k p) n -> p k n", p=128)
    for pc in range(24):
        sl = pc % 2
        P.add("sp", lambda e, pc=pc, sl=sl: e.dma_start(out=wst[:, sl], in_=w_ada_v[:, :, pc * 256:(pc + 1) * 256]),
              w=[("wst", sl)], dma_key=("wst", sl))
        for sub in range(2):
            blk = pc * 2 + sub
            for kc in range(8):
                P.add("pe", lambda e, sl=sl, sub=sub, kc=kc, blk=blk: e.matmul(
                    ps[bk_ada][:, blk * 4:blk * 4 + 4], lhsT=wst[:, sl, kc, sub * 128:(sub + 1) * 128],
                    rhs=cactT[:, kc * 4:kc * 4 + 4], start=(kc == 0), stop=(kc == 7)),
                    r=[("wst", sl), "cactT"], w=[("ps", bk_ada)])
    P.add("dve", lambda e: e.tensor_tensor(out=adaT[:], in0=ps[bk_ada][:, 0:192].rearrange("p (k b) -> p k b", b=4),
                                           in1=A(badaT, 0, [[48, 128], [1, 48], [0, 4]]), op=ALU.add),
          r=[("ps", bk_ada), "badaT"], w=["adaT"])
    for (GT, sc0, q) in ((G1T, 8, 0), (G2T, 32, 1)):
        P.add("dve", lambda e, GT=GT, sc0=sc0: e.tensor_scalar(out=GT[:], in0=adaT[:, sc0:sc0 + 8, :], scalar1=1.0,
                                                               scalar2=None, op0=ALU.add), r=["adaT"], w=[("G", q)])
        P.add("dve", lambda e, GT=GT, q=q: e.tensor_tensor(out=GT[:], in0=GT[:], in1=A(gT, q * 8, [[16, 128], [1, 8], [0, 4]]),
                                                           op=ALU.mult), r=[("G", q), "gT"], w=[("G", q)])
    dump("adaT", adaT[:].rearrange("p k b -> p (k b)"), ["adaT"])

    for h in range(8 if stop_after != "pro_a" else 0):
        Gtab = Gtabs[h % 2]
        gres = ("Gtab", h % 2)
        P.add("dve", lambda e, h=h: e.tensor_scalar(out=rbx[:], in0=cf[0:33, 128:256], scalar1=rb33[0:33, h:h + 1],
                                                    scalar2=None, op0=ALU.mult),
              r=["cf", "rb33a", "rb33b"], w=["rbx"])
        for half in range(2):
            b_ = nbank()
            P.add("pe", lambda e, half=half, b_=b_: e.matmul(ps[b_][:, 0:384], lhsT=rbx[0:33, :],
                                                             rhs=ehot_sb[:, half * 384:(half + 1) * 384], start=True, stop=True),
                  r=["rbx", "ehot"], w=[("ps", b_)])
            P.add("act", lambda e, half=half, b_=b_, Gtab=Gtab: e.activation(out=Gtab[:, half * 384:(half + 1) * 384],
                                                                            in_=ps[b_][:, 0:384], func=AF.Exp),
                  r=[("ps", b_)], w=[gres])
        P.add("sp", lambda e, h=h, Gtab=Gtab: e.dma_start(out=A(toep, h * TOEPN, [[LT, 128], [1, LT]]), in_=Gtab),
              r=[gres], w=[("toep", h)], dma_key=("toepw", h % 2))
        P.add("pool", lambda e, h=h: e.dma_start(out=Mown[:, h, :], in_=A(toep, h * TOEPN + 255, [[LT - 1, 128], [1, 256]])),
              r=[("toep", h)], w=[("Mown", h)], dma_key=("toepr", h))
        P.add("pool", lambda e, h=h: e.dma_start(out=Mprev[:, h, :], in_=A(toep, h * TOEPN + 383, [[LT - 1, 128], [1, 112]])),
              r=[("toep", h)], w=[("Mprev", h)], dma_key=("toepr2", h))

    wctr = [0]

    def wload(src_ap, wb_name, k, n):
        sl = wctr[0] % 2
        wctr[0] += 1
        view = wslot[sl][:, 0:k * n].rearrange("p (k n) -> p k n", n=n)
        res = ("wslot", sl)
        P.add("sp", lambda e: e.dma_start(out=view, in_=src_ap), r=WB_RES[wb_name], w=[res], dma_key=res)
        return view, res

    wb_in_v = wb_in.ap().rearrange("(k p) n -> p k n", p=128)
    wb_out_v = wb_out.ap().rearrange("(k p) n -> p k n", p=128)
    wb_1_v = wb_1.ap().rearrange("(k p) n -> p k n", p=128)
    wb_2_v = wb_2.ap().rearrange("(k p) n -> p k n", p=128)

    def norm_stats():
        for tt in range(NT):
            P.add("act", lambda e, tt=tt: e.activation(out=junk, in_=xblk[:, tt, :], func=AF.Square, scale=1.0 / 32.0,
                                                       accum_out=ms[:, tt:tt + 1]),
                  r=[("x", tt)], w=JUNK_RES + [("ms", tt)])
            P.add("act", lambda e, tt=tt: e.activation(out=rstd[:, tt:tt + 1], in_=ms[:, tt:tt + 1], func=AF.Ln, bias=EPS, scale=1.0),
                  r=[("ms", tt)], w=[("rstd", tt)])
            P.add("act", lambda e, tt=tt: e.activation(out=rstd[:, tt:tt + 1], in_=rstd[:, tt:tt + 1], func=AF.Exp, scale=-0.5),
                  r=[("rstd", tt)], w=[("rstd", tt)])

    def norm_hT(b, which):
        GT = G1T if which == 0 else G2T
        gq_ = ("G", which)
        sh0 = 0 if which == 0 else 24
        norm_stats()
        for half in range(2):
            banks = [nbank() for _ in range(4)]
            for tt in range(NT):
                if half == 0:
                    P.add("dve", lambda e, tt=tt: e.tensor_scalar(out=xsfull[:, tt, :], in0=xblk[:, tt, :],
                                                                  scalar1=rstd[:, tt:tt + 1], scalar2=None, op0=ALU.mult),
                          r=[("x", tt), ("rstd", tt), "adaT"], w=XS_RES[tt])
                for q in range(4):
                    kc = half * 4 + q
                    P.add("pe", lambda e, tt=tt, kc=kc, bq=banks[q]: e.matmul(
                        ps[bq][:, tt * 128:(tt + 1) * 128], lhsT=xsfull[:, tt, kc * 128:(kc + 1) * 128], rhs=ident_b,
                        start=True, stop=True), r=XS_RES[tt] + ["cb"], w=[("ps", banks[q])])
            for q in range(4):
                kc = half * 4 + q
                bq = banks[q]
                if q % 2 == 0:
                    P.add("act", lambda e, kc=kc, bq=bq: e.activation(out=hT[:, kc, :], in_=ps[bq][:, :], func=AF.Identity,
                                                                     scale=GT[:, kc, b:b + 1], bias=adaT[:, sh0 + kc, b:b + 1]),
                          r=[("ps", bq), gq_, "adaT"], w=[("hT", kc)])
                else:
                    P.add("dve", lambda e, kc=kc, bq=bq: e.tensor_scalar(out=hT[:, kc, :], in0=ps[bq][:, :],
                                                                        scalar1=GT[:, kc, b:b + 1], scalar2=adaT[:, sh0 + kc, b:b + 1],
                                                                        op0=ALU.mult, op1=ALU.add),
                          r=[("ps", bq), gq_, "adaT"], w=[("hT", kc)])

    def acc_mm(out_ap, pairs, r, w):
        n = len(pairs)
        for i_, (l, rh) in enumerate(pairs):
            P.add("pe", lambda e, l=l, rh=rh, i_=i_: e.matmul(out_ap, lhsT=l, rhs=rh, start=(i_ == 0), stop=(i_ == n - 1)),
                  r=r, w=w)

    HT_ALL = [("hT", kc) for kc in range(8)]
    first = [True]

    class _Stop(Exception):
        pass

    def chk(name):
        if stop_after == name:
            raise _Stop()

    out_ops = []
    try:
      for b in range(nseq):
          if stop_after in ("prologue", "pro_a", "pro_b"):
              break
          for (gt_, blk0, nm) in ((gate_a, 16, "gate_a"), (gate_m, 40, "gate_m")):
              for half in range(2):
                  bk = nbank()
                  for q in range(4):
                      kc = half * 4 + q
                      dg = diag[kc % 2]
                      P.add("dve", lambda e, dg=dg, kc=kc, blk0=blk0, b=b: e.tensor_scalar(out=dg[:], in0=ident_f,
                                                                                     scalar1=adaT[:, blk0 + kc, b:b + 1], scalar2=None,
                                                                                     op0=ALU.mult),
                            r=["cf", "adaT"], w=[("diag", kc % 2)])
                      P.add("pe", lambda e, dg=dg, q=q, bk=bk: e.matmul(ps[bk][:, q * 128:(q + 1) * 128], lhsT=ones_f, rhs=dg[:],
                                                                        start=True, stop=True),
                            r=[("diag", kc % 2), "cf"], w=[("ps", bk)])
                  P.add("act", lambda e, gt_=gt_, half=half, bk=bk: e.activation(out=gt_[:, half * 512:(half + 1) * 512],
                                                                                in_=ps[bk][:, :], func=AF.Copy),
                        r=[("ps", bk), "cactT"], w=[(nm, half)])
          P.add("dve", lambda e: e.memset(Sf[:], 0.0), w=["Sf"])
          P.add("dve", lambda e: e.memset(Sb[:], 0.0), w=["Sb"])
          P.add("dve", lambda e: e.memset(kn2r[:], 0.0), w=["kn2r"])

          for T in range(nblk):
              i0, i1 = 2 * T, 2 * T + 1
              isdbg = first[0]
              first[0] = False
              for tt in range(NT):
                  xsrc = x_d.ap()[b, T * TB + tt * 128:T * TB + (tt + 1) * 128, :]
                  P.add("sp", lambda e, xsrc=xsrc, tt=tt: e.dma_start(out=xblk[:, tt, :], in_=xsrc), w=[("x", tt)],
                        dma_key=("xload", tt))
              norm_hT(b, 0)
              if isdbg:
                  dump("hT", hT[:].rearrange("p k t -> p (k t)"), HT_ALL)
              if stop_after == "norm":
                  break
              wv, wr = wload(wb_in_v[:, :, 0:512], "wb_in", 8, 512)
              for cbk in range(4):
                  bk = nbank()
                  acc_mm(ps[bk][:, :], [(wv[:, kc, cbk * 128:(cbk + 1) * 128], hT[:, kc, :]) for kc in range(8)],
                         r=[wr] + HT_ALL, w=[("ps", bk)])
                  if cbk < 2:
                      P.add("act", lambda e, cbk=cbk, bk=bk: e.activation(out=gqT[:, cbk, :], in_=ps[bk][:, :], func=AF.Identity, scale=0.125),
                            r=[("ps", bk)], w=[("gqT", cbk)])
                  else:
                      P.add("dve", lambda e, cbk=cbk, bk=bk: e.tensor_copy(out=gkT[:, cbk - 2, :], in_=ps[bk][:, :]),
                            r=[("ps", bk)], w=[("gkT", cbk - 2)])
              for tt in range(NT):
                  bk = nbank()
                  acc_mm(ps[bk][:, 0:256], [(hT[:, kc, tt * 128:(tt + 1) * 128], wv[:, kc, 256:512]) for kc in range(8)],
                         r=[wr] + HT_ALL, w=[("ps", bk)])
                  P.add("act", lambda e, tt=tt, bk=bk: e.activation(out=gktok[:, tt, :], in_=ps[bk][:, 0:256], func=AF.Copy),
                        r=[("ps", bk)], w=[("gktok", tt)])
              chk("ip0")
              wv, wr = wload(wb_in_v[:, :, 512:1024], "wb_in", 8, 512)
              for tt in range(NT):
                  bk = nbank()
                  acc_mm(ps[bk][:, :], [(hT[:, kc, tt * 128:(tt + 1) * 128], wv[:, kc, :]) for kc in range(8)],
                         r=[wr] + HT_ALL, w=[("ps", bk)])
                  P.add("dve", lambda e, tt=tt, bk=bk: e.tensor_copy(out=gv[:, tt, :], in_=ps[bk][:, :]),
                        r=[("ps", bk)], w=[("gv", tt)])
              chk("ip1")
              wv, wr = wload(wb_in_v[:, :, 1024:1536], "wb_in", 8, 512)
              for tt in range(NT):
                  bk = nbank()
                  acc_mm(ps[bk][:, :], [(hT[:, kc, tt * 128:(tt + 1) * 128], wv[:, kc, :]) for kc in range(8)],
                         r=[wr] + HT_ALL, w=[("ps", bk)])
                  P.add("act", lambda e, bk=bk: e.activation(out=sig[:], in_=ps[bk][:, :], func=AF.Exp, scale=-1.0),
                        r=[("ps", bk)], w=["sig"])
                  P.add("dve", lambda e, tt=tt, bk=bk: e.tensor_tensor(out=t3[:, tt, :], in0=ps[bk][:, :], in1=gout_bc[:], op=ALU.mult),
                        r=[("ps", bk), "gout_bc"], w=[("t3", tt)])
                  P.add("act", lambda e: e.activation(out=sig[:], in_=sig[:], func=AF.Ln, bias=1.0, scale=1.0), r=["sig"], w=["sig"])
                  P.add("act", lambda e: e.activation(out=sig[:], in_=sig[:], func=AF.Exp, scale=-1.0), r=["sig"], w=["sig"])
                  P.add("dve", lambda e, tt=tt: e.tensor_tensor(out=t3[:, tt, :], in0=t3[:, tt, :], in1=sig[:], op=ALU.mult),
                        r=["sig", ("t3", tt)], w=[("t3", tt)])
              chk("ip2")
              wvg, wrg = wload(wb_in_v[:, :, 1536:1552], "wb_in", 8, 16)
              bk = nbank()
              acc_mm(ps[bk][0:16, :], [(wvg[:, kc, 0:16], hT[:, kc, :]) for kc in range(8)], r=[wrg] + HT_ALL, w=[("ps", bk)])
              P.add("act", lambda e, bk=bk: e.activation(out=ggT[0:16, :], in_=ps[bk][0:16, :], func=AF.Copy),
                    r=[("ps", bk)], w=["ggT"])
              chk("ip3")
              def ipm_gen():
                  wv, wr = wload(wb_in_v[:, :, 1552:2064], "wb_in", 8, 512)
                  for pr in range(4):
                      bk = nbank(ringM)
                      acc_mm(ps[bk][:, :], [(wv[:, kc, pr * 128:(pr + 1) * 128], hT[:, kc, :]) for kc in range(8)],
                             r=[wr] + HT_ALL, w=[("ps", bk)])
                      P.add("act", lambda e, pr=pr, bk=bk: e.activation(out=Qt[:, pr, :], in_=ps[bk][:, :], func=AF.Identity, scale=0.125),
                            r=[("ps", bk)], w=[("Qt", pr)])
                      yield
                  wv, wr = wload(wb_in_v[:, :, 2064:2576], "wb_in", 8, 512)
                  for pr in range(4):
                      bk = nbank(ringM)
                      acc_mm(ps[bk][:, :], [(wv[:, kc, pr * 128:(pr + 1) * 128], hT[:, kc, :]) for kc in range(8)],
                             r=[wr] + HT_ALL, w=[("ps", bk)])
                      P.add("dve", lambda e, pr=pr, bk=bk, T=T: e.tensor_copy(out=Kt[:, pr, T * TB:(T + 1) * TB], in_=ps[bk][:, :]),
                            r=[("ps", bk)], w=[("Kt", pr, T)])
                      yield
                  wv, wr = wload(wb_in_v[:, :, 2576:3088], "wb_in", 8, 512)
                  for tt in range(NT):
                      g_t = T * NT + tt
                      bk = nbank(ringM)
                      acc_mm(ps[bk][:, :], [(hT[:, kc, tt * 128:(tt + 1) * 128], wv[:, kc, :]) for kc in range(8)],
                             r=[wr] + HT_ALL, w=[("ps", bk)])
                      P.add("act", lambda e, g_t=g_t, bk=bk: e.activation(
                          out=A(Vint, g_t * 768, [[16 * 768, 128], [192, 4], [1, 64]]),
                          in_=A(ps[bk], 0, [[512, 128], [128, 4], [1, 64]]), func=AF.Copy),
                          r=[("ps", bk)], w=[("V", g_t)])
                      P.add("dve", lambda e, g_t=g_t, bk=bk: e.tensor_copy(
                          out=A(Vint, g_t * 768 + 128, [[16 * 768, 128], [192, 4], [1, 64]]),
                          in_=A(ps[bk], 64, [[512, 128], [128, 4], [1, 64]])),
                          r=[("ps", bk), ("V", g_t)], w=[("V", g_t)])
                      yield
              if isdbg:
                  dump("gqT", gqT[:].rearrange("p k t -> p (k t)"), [("gqT", 0), ("gqT", 1)])
                  dump("gktok", gktok[:].rearrange("p k t -> p (k t)"), [("gktok", t_) for t_ in range(NT)])
                  dump("t3", t3[:].rearrange("p k t -> p (k t)"), [("t3", t_) for t_ in range(NT)])
                  dump("Qt", Qt[:].rearrange("p k t -> p (k t)"), [("Qt", p_) for p_ in range(4)])
                  dump("Kt", Kt[:, :, 0:TB], [("Kt", p_, 0) for p_ in range(4)])
                  dump("Vint", Vint[:, 0:4, :].rearrange("p k t -> p (k t)"), [("V", t_) for t_ in range(4)])
              if stop_after == "inproj":
                  break

              def gla_gen():
                  def gla_front(tt):
                      csl = slice(tt * 128, (tt + 1) * 128)
                      bz = nbank(ringG)
                      P.add("pe", lambda e, bz=bz, csl=csl: e.matmul(ps[bz][:, 0:256], lhsT=ggT[0:17, csl], rhs=wg[0:17, :], start=True, stop=True),
                            r=["ggT", "wg0", "wg1"], w=[("ps", bz)])
                      P.add("act", lambda e, bz=bz: e.activation(out=Lg[:], in_=ps[bz][:, 0:256], func=AF.Exp, scale=-1.0),
                            r=[("ps", bz)], w=["Lg"])
                      P.add("act", lambda e: e.activation(out=Lg[:], in_=Lg[:], func=AF.Ln, bias=1.0, scale=1.0), r=["Lg"], w=["Lg"])
                      yield
                      bc_ = nbank(ringG)
                      bt_ = bc_
                      P.add("pe", lambda e, bc_=bc_: e.matmul(ps[bc_][:, 0:256], lhsT=cf[:, 384:512], rhs=Lg[:], start=True, stop=True),
                            r=["Lg", "cf"], w=[("ps", bc_)])
                      for blk in range(2):
                          P.add("pe", lambda e, bt_=bt_, blk=blk: e.matmul(ps[bt_][:, 256 + blk * 128:256 + (blk + 1) * 128],
                                                                           lhsT=Lg[:, blk * 128:(blk + 1) * 128], rhs=cf[:, 256:384],
                                                                           start=True, stop=True),
                                r=["Lg", "cf"], w=[("ps", bt_)])
                      bT3 = ps[bt_][:, 256:512].rearrange("p (k t) -> p k t", t=128)
                      P.add("dve", lambda e, bT3=bT3: e.tensor_copy(out=brf[:], in_=bT3[:, :, 63]), r=[("ps", bt_)], w=["brf"])
                      P.add("dve", lambda e, bT3=bT3: e.tensor_scalar(out=nbrf[:], in0=bT3[:, :, 63], scalar1=-1.0, scalar2=None, op0=ALU.mult),
                            r=[("ps", bt_)], w=["nbrf"])
                      P.add("act", lambda e, bT3=bT3: e.activation(out=dec2[tt % 2][:], in_=bT3[:, :, 127], func=AF.Exp), r=[("ps", bt_)], w=[("dec", tt % 2)])
                      for blk in range(2):
                          P.add("act", lambda e, blk=blk, bT3=bT3: e.activation(out=eA[:, blk, :], in_=bT3[:, blk, :], func=AF.Exp,
                                                                               bias=nbrf[:, blk:blk + 1], scale=1.0),
                                r=[("ps", bt_), "nbrf"], w=[("eA", blk)])
                          P.add("act", lambda e, blk=blk, bT3=bT3: e.activation(out=eK[:, blk, :], in_=bT3[:, blk, :], func=AF.Exp,
                                                                               bias=brf[:, blk:blk + 1], scale=-1.0),
                                r=[("ps", bt_), "brf"], w=[("eK", blk)])
                      P.add("act", lambda e, bT3=bT3: e.activation(out=eB[:], in_=bT3, func=AF.Exp), r=[("ps", bt_)], w=["eB"])
                      P.add("act", lambda e, bc_=bc_: e.activation(out=eC[:], in_=ps[bc_][:, 0:256], func=AF.Exp), r=[("ps", bc_)], w=["eC"])
                      yield
                      for par in range(2):
                          rs = slice(64 * par, 64 * par + 64)
                          P.add("dve", lambda e, csl=csl, par=par, rs=rs: e.tensor_tensor(out=qAz[par][rs, :, :], in0=gqT[rs, :, csl], in1=eA[rs, :, :], op=ALU.mult),
                                r=[("gqT", 0), ("gqT", 1), ("eA", 0), ("eA", 1)], w=[("qA", par)])
                          P.add("dve", lambda e, csl=csl, par=par, rs=rs: e.tensor_tensor(out=qBz2[tt % 2][par][rs, :, :], in0=gqT[rs, :, csl], in1=eB[rs, :, :], op=ALU.mult),
                                r=[("gqT", 0), ("gqT", 1), "eB"], w=[("qB", tt % 2, par)])
                      P.add("dve", lambda e, csl=csl: e.tensor_tensor(out=kA[:], in0=gkT[:, :, csl], in1=eK[:], op=ALU.mult),
                            r=[("gkT", 0), ("gkT", 1), ("eK", 0), ("eK", 1)], w=["kA"])
                      P.add("dve", lambda e, tt=tt: e.tensor_tensor(out=kC2[tt % 2][:], in0=gktok[:, tt, :], in1=eC[:], op=ALU.mult),
                            r=[("gktok", tt), "eC"], w=[("kC", tt % 2)])
                      yield
                      ba = nbank(ringG)
                      for h in range(4):
                          blk, r0 = h // 2, 64 * (h % 2)
                          P.add("pe", lambda e, ba=ba, h=h, blk=blk, r0=r0: e.matmul(ps[ba][:, h * 128:(h + 1) * 128],
                                                                                     lhsT=kA[:, blk, :], rhs=qAz[h % 2][:, blk, :],
                                                                                     start=True, stop=True),
                                r=["kA", ("qA", h % 2)], w=[("ps", ba)])
                      P.add("dve", lambda e, ba=ba: e.tensor_tensor(out=attT2[tt % 2][:], in0=ps[ba][:, :].rearrange("p (h t) -> p h t", t=128),
                                                                    in1=A(cb, 128, [[288, 128], [0, 4], [1, 128]]), op=ALU.mult),
                            r=[("ps", ba), "cb"], w=[("attT", tt % 2)])
                      yield
                  def gla_back(tt):
                      csl = slice(tt * 128, (tt + 1) * 128)
                      bo = nbank(ringB)
                      for h in range(4):
                          blk, r0 = h // 2, 64 * (h % 2)
                          P.add("pe", lambda e, bo=bo, h=h, tt=tt: e.matmul(ps[bo][:, h * 128:(h + 1) * 128], lhsT=attT2[tt % 2][:, h, :],
                                                                            rhs=gv[:, tt, h * 128:(h + 1) * 128], start=True, stop=False),
                                r=[("attT", tt % 2), ("gv", tt)], w=[("ps", bo)])
                          P.add("pe", lambda e, bo=bo, h=h, blk=blk, r0=r0: e.matmul(ps[bo][:, h * 128:(h + 1) * 128],
                                                                                     lhsT=qBz2[tt % 2][h % 2][:, blk, :], rhs=Sb[:, blk, :],
                                                                                     start=False, stop=True),
                                r=[("qB", tt % 2, h % 2), "Sb"], w=[("ps", bo)])
                      yield
                      bkv = nbank(ringB)
                      for h in range(4):
                          blk, r0 = h // 2, 64 * (h % 2)
                          P.add("pe", lambda e, bkv=bkv, h=h, blk=blk, r0=r0, tt=tt: e.matmul(
                              ps[bkv][r0:r0 + 64, blk * 128:(blk + 1) * 128], lhsT=kC2[tt % 2][:, h * 64:(h + 1) * 64],
                              rhs=gv[:, tt, h * 128:(h + 1) * 128], start=True, stop=True),
                              r=[("kC", tt % 2), ("gv", tt)], w=[("ps", bkv)])
                      for blk in range(2):
                          P.add("dve", lambda e, bkv=bkv, blk=blk: e.scalar_tensor_tensor(
                              out=Sf[:, blk, :], in0=Sf[:, blk, :], scalar=dec2[tt % 2][:, blk:blk + 1], in1=ps[bkv][:, blk * 128:(blk + 1) * 128],
                              op0=ALU.mult, op1=ALU.add), r=[("ps", bkv), ("dec", tt % 2), "Sf"], w=["Sf"])
                      P.add("act", lambda e: e.activation(out=Sb[:], in_=Sf[:], func=AF.Copy), r=["Sf"], w=["Sb"])
                      yield
                      for h in range(4):
                          P.add("act", lambda e, bo=bo, h=h: e.activation(out=junk[:, 0:128], in_=ps[bo][:, h * 128:(h + 1) * 128], func=AF.Square,
                                                                         scale=float(128.0 ** -0.5), accum_out=oms[:, h:h + 1]),
                                r=[("ps", bo)], w=JUNK_RES + ["oms"])
                      P.add("act", lambda e: e.activation(out=orst[:], in_=oms[:], func=AF.Ln, bias=EPS, scale=1.0),
                            r=["oms"], w=["orst"])
                      P.add("act", lambda e: e.activation(out=orst[:], in_=orst[:], func=AF.Exp, scale=-0.5), r=["orst"], w=["orst"])
                      for h in range(4):
                          P.add("dve", lambda e, bo=bo, h=h, tt=tt: e.scalar_tensor_tensor(
                              out=on_[:, h * 128:(h + 1) * 128], in0=ps[bo][:, h * 128:(h + 1) * 128], scalar=orst[:, h:h + 1],
                              in1=t3[:, tt, h * 128:(h + 1) * 128], op0=ALU.mult, op1=ALU.mult),
                              r=[("ps", bo), "orst", ("t3", tt)], w=["on"])
                      yield
                      btr = nbank(ringB)
                      for h in range(4):
                          P.add("pe", lambda e, btr=btr, h=h: e.matmul(ps[btr][:, h * 128:(h + 1) * 128], lhsT=on_[:, h * 128:(h + 1) * 128],
                                                                       rhs=ident_b, start=True, stop=True),
                                r=["on", "cb"], w=[("ps", btr)])
                      P.add("act", lambda e, btr=btr, csl=csl: e.activation(out=attnT[:, 0:4, csl],
                                                                           in_=ps[btr][:, :].rearrange("p (h t) -> p h t", t=128), func=AF.Copy),
                            r=[("ps", btr)], w=[("attnT_g", tt)])
                      yield
                  if T >= 2:
                      for tt_ in range(NT):
                          yield from gla_front(tt_)
                          yield from gla_back(tt_)
                      return
                  yield from gla_front(0)
                  for tt_ in range(NT):
                      gens = [gla_back(tt_)] + ([gla_front(tt_ + 1)] if tt_ + 1 < NT else [])
                      while gens:
                          for g_ in list(gens):
                              try:
                                  next(g_)
                              except StopIteration:
                                  gens.remove(g_)
                          yield
              def moba_gen():
                  P.add("dve", lambda e, T=T: e.tensor_reduce(out=kmsum[:, :, 2 * T:2 * T + 2],
                                                         in_=Kt[:, :, T * TB:(T + 1) * TB].rearrange("p a (j s) -> p a j s", s=256),
                                                         axis=AX.X, op=ALU.add),
                        r=[("Kt", pr, T) for pr in range(4)], w=["kmsum"])
                  P.add("act", lambda e: e.activation(out=kmean[:], in_=kmsum[:], func=AF.Identity, scale=1.0 / 256.0), r=["kmsum"], w=["kmean"])
                  for (src, dst, nm) in ((Qt[:, :, :], qn2, "qn2"), (Kt[:, :, T * TB:(T + 1) * TB], kn2, "kn2")):
                      rr = [("Qt", pr) for pr in range(4)] if nm == "qn2" else [("Kt", pr, T) for pr in range(4)]
                      P.add("act", lambda e, src=src: e.activation(out=sq, in_=src, func=AF.Square), r=rr, w=SQ_RES)
                      bn = nbank(ringM)
                      for pr in range(4):
                          P.add("pe", lambda e, bn=bn, pr=pr: e.matmul(ps[bn][0:8, :], lhsT=cb[:, 256 + pr * 8:264 + pr * 8],
                                                                       rhs=sq[:, pr, :], start=(pr == 0), stop=(pr == 3)),
                                r=SQ_RES + ["cb"], w=[("ps", bn)])
                      P.add("dve", lambda e, bn=bn, dst=dst: e.tensor_reduce(out=dst[:], in_=ps[bn][0:8, :], axis=AX.X, op=ALU.max),
                            r=[("ps", bn)], w=[nm])
                  P.add("dve", lambda e: e.tensor_tensor(out=kn2r[:], in0=kn2r[:], in1=kn2[:], op=ALU.max), r=["kn2", "kn2r"], w=["kn2r"])
                  P.add("dve", lambda e: e.tensor_tensor(out=bnd[:], in0=qn2[:], in1=kn2r[:], op=ALU.mult), r=["qn2", "kn2r"], w=["bnd"])
                  P.add("act", lambda e: e.activation(out=bnd[:], in_=bnd[:], func=AF.Ln, bias=EPS, scale=1.0), r=["bnd"], w=["bnd"])
                  P.add("act", lambda e: e.activation(out=bnd[:], in_=bnd[:], func=AF.Exp, scale=0.5), r=["bnd"], w=["bnd"])
                  P.add("dve", lambda e: e.tensor_scalar(out=bnd8[:], in0=cf[0:8, 1024:1032], scalar1=bnd[:, 0:1], scalar2=None, op0=ALU.mult),
                        r=["bnd", "cf"], w=["bnd8"])
                  bb = nbank(ringM)
                  P.add("pe", lambda e, bb=bb: e.matmul(ps[bb][:, 0:8], lhsT=cf[0:8, 128:256], rhs=bnd8[:], start=True, stop=True),
                        r=["bnd8", "cf"], w=[("ps", bb)])
                  P.add("dve", lambda e, bb=bb: e.tensor_scalar(out=nbias[:], in0=ps[bb][:, 0:8], scalar1=-1.03, scalar2=-0.5,
                                                                op0=ALU.mult, op1=ALU.add), r=[("ps", bb)], w=["nbias"])
                  P.add("dve", lambda e: e.tensor_tensor(out=nbias[:], in0=nbias[:], in1=b31bc[:], op=ALU.add), r=["nbias", "b31bc"], w=["nbias"])

                  need_mask = (T >= 2)
                  if need_mask:
                      for tt in range(NT):
                          own = 2 * T + tt // 2
                          bgs = [nbank(ringM), nbank(ringM)]
                          for h in range(8):
                              pr, r0 = h // 2, 64 * (h % 2)
                              bg = bgs[h % 2]
                              P.add("pe", lambda e, bg=bg, h=h, pr=pr, r0=r0, tt=tt: e.matmul(
                                  ps[bg][:, pr * 8:(pr + 1) * 8], lhsT=Qt[r0:r0 + 64, pr, tt * 128:(tt + 1) * 128],
                                  rhs=kmean[r0:r0 + 64, pr, :], start=True, stop=True),
                                  r=[("Qt", pr), "kmean"], w=[("ps", bg)])
                          for par in range(2):
                              P.add("dve", lambda e, bg=bgs[par], own=own, par=par: e.tensor_tensor(
                                  out=A(gmx, par * 8, [[64, 128], [16, 4], [1, 8]]),
                                  in0=ps[bg][:, 0:32].rearrange("p (a n) -> p a n", n=8),
                                  in1=cf[:, 512 + own * 64:512 + own * 64 + 32].rearrange("p (a n) -> p a n", n=8), op=ALU.add),
                                  r=[("ps", bgs[par]), "cf"], w=["gmx"])
                          for h in range(8):
                              P.add("dve", lambda e, h=h: e.max(out=m8[:, h, :], in_=gmx[:, h * 8:(h + 1) * 8]), r=["gmx"], w=["m8"])
                          P.add("dve", lambda e: e.tensor_tensor(out=selt[:].rearrange("p (h n) -> p h n", n=8),
                                                                 in0=gmx[:].rearrange("p (h n) -> p h n", n=8),
                                                                 in1=A(m8, 3, [[64, 128], [8, 8], [0, 8]]), op=ALU.is_ge),
                                r=["gmx", "m8"], w=["selt"])
                          P.add("dve", lambda e, tt=tt: e.tensor_scalar(out=mrow[:, tt, :], in0=selt[:], scalar1=-1.0, scalar2=BIG,
                                                                        op0=ALU.add, op1=ALU.mult), r=["selt"], w=[("mrow", tt)])

                  LOOK = 1
                  tl = []
                  for j in range(i1 + 1):
                      for c in range(2):
                          if j < i0:
                              lo = 0
                          elif j == i0:
                              lo = 128 * c
                          else:
                              lo = 256 + 128 * c
                          tl.append((j, c, lo))
                  items = [(pr, ti, j, c, lo) for pr in range(4) for ti, (j, c, lo) in enumerate(tl)]
                  ntl = len(tl)

                  def mask_prep(pr):
                      bm = nbank(ringM)
                      for par in range(2):
                          h = 2 * pr + par
                          r0 = 64 * par
                          for tt in range(NT):
                              P.add("pe", lambda e, bm=bm, tt=tt, h=h, r0=r0: e.matmul(ps[bm][r0:r0 + 8, tt * 128:(tt + 1) * 128],
                                                                                       lhsT=mrow[:, tt, h * 8:(h + 1) * 8], rhs=ident_b,
                                                                                       start=True, stop=True),
                                    r=[("mrow", tt), "cb"], w=[("ps", bm)])
                      mT = maskT[pr % 2]
                      for par in range(2):
                          r0 = 64 * par
                          P.add("act", lambda e, bm=bm, mT=mT, r0=r0: e.activation(out=mT[r0:r0 + 8, :], in_=ps[bm][r0:r0 + 8, :], func=AF.Copy),
                                r=[("ps", bm)], w=[("maskT", pr % 2, par)])

                  if need_mask:
                      mask_prep(0)
                  yield
                  for idx in range(len(items) + LOOK):
                      if idx < len(items):
                          pr, ti, j, c, lo = items[idx]
                          mT = maskT[pr % 2]
                          if need_mask and ti == 0 and pr + 1 < 4:
                              mask_prep(pr + 1)
                          kt = 2 * j + c
                          use_mask = need_mask and (j < i1)
                          bss = [nbank(ringM), nbank(ringM)]
                          for par in range(2):
                              r0 = 64 * par
                              P.add("pe", lambda e, bs_=bss[par], kt=kt, lo=lo, pr=pr, r0=r0, use_mask=use_mask: e.matmul(
                                  ps[bs_][:, lo:TB], lhsT=Kt[r0:r0 + 64, pr, kt * 128:(kt + 1) * 128], rhs=Qt[r0:r0 + 64, pr, lo:TB],
                                  start=True, stop=(not use_mask)),
                                  r=[("Kt", pr, kt // 4), ("Qt", pr)], w=[("ps", bss[par])])
                          if use_mask:
                              for par in range(2):
                                  r0 = 64 * par
                                  P.add("pe", lambda e, bs_=bss[par], j=j, lo=lo, mT=mT, r0=r0: e.matmul(
                                      ps[bs_][:, lo:TB], lhsT=ohk[r0:r0 + 8, j * 128:(j + 1) * 128], rhs=mT[r0:r0 + 8, lo:TB], start=False, stop=True),
                                      r=["ohk", "ohk2", ("maskT", pr % 2, par)], w=[("ps", bss[par])])
                          for par in range(2):
                              h = 2 * pr + par
                              pi = (2 * idx + par) % NPT
                              pt = Pt[pi]
                              pres = ("Pt", pi)
                              P.add("act", lambda e, bs_=bss[par], lo=lo, pt=pt, h=h: e.activation(out=pt[:, lo:TB], in_=ps[bs_][:, lo:TB], func=AF.Exp,
                                                                                                 bias=nbias[:, h:h + 1], scale=1.0),
                                    r=[("ps", bss[par]), "nbias"], w=[pres])
                              fix = []
                              if j == i0:
                                  fix.append((lo, 256, Mown[:, h, 0:256 - lo]))
                                  if c == 1:
                                      fix.append((256, 368, Mprev[:, h, :]))
                              elif j == i1:
                                  fix.append((lo, 512, Mown[:, h, 0:512 - lo]))
                              elif j == i0 - 1 and c == 1:
                                  fix.append((0, 112, Mprev[:, h, :]))
                              for (a0, a1, tab) in fix:
                                  P.add("dve", lambda e, pt=pt, a0=a0, a1=a1, tab=tab: e.tensor_tensor(out=pt[:, a0:a1], in0=pt[:, a0:a1], in1=tab,
                                                                                                      op=ALU.mult),
                                        r=[pres, ("Mown", h), ("Mprev", h)], w=[pres])
                      if idx - LOOK >= 0:
                          pr, ti, j, c, lo = items[idx - LOOK]
                          kt = 2 * j + c
                          for par in range(2):
                              h = 2 * pr + par
                              r0 = 64 * par
                              dr0 = 64 - r0
                              acc = 6 + par
                              pi = (2 * (idx - LOOK) + par) % NPT
                              pt = Pt[pi]
                              pres = ("Pt", pi)
                              vcol = pr * 192 + (0 if par == 0 else 64)
                              P.add("pe", lambda e, acc=acc, kt=kt, lo=lo, pt=pt, vcol=vcol, ti=ti, ntl=ntl: e.matmul(
                                  ps[acc][:, lo:TB], lhsT=Vint[:, kt, vcol:vcol + 128], rhs=pt[:, lo:TB],
                                  start=(ti == 0), stop=(ti == ntl - 1), skip_group_check=True),
                                  r=[pres, ("V", kt)], w=[("ps", acc)])
                              if ti == ntl - 1:
                                  rd = rden2[par]
                                  rres = ("rden", par)
                                  P.add("dve", lambda e, acc=acc, r0=r0, dr0=dr0, rd=rd: e.reciprocal(out=rd[r0:r0 + 64, :], in_=ps[acc][dr0:dr0 + 64, :]),
                                        r=[("ps", acc)], w=[rres])
                                  P.add("dve", lambda e, acc=acc, r0=r0, pr=pr, rd=rd: e.tensor_tensor(out=attnT[r0:r0 + 64, 4 + pr, :], in0=ps[acc][r0:r0 + 64, :],
                                                                                                in1=rd[r0:r0 + 64, :], op=ALU.mult),
                                        r=[("ps", acc), rres], w=[("attnT_m", h)])
                      yield
              if T < 2:
                  ringG.banks, ringB.banks, ringM.banks = [0], [1, 2], [3, 4, 5]
              else:
                  ringG.banks, ringB.banks, ringM.banks = [0], [0, 1], [2, 3, 4, 5]
              ringG.reset()
              ringB.reset()
              ringM.reset()
              gg_ = gla_gen()
              mg_ = moba_gen()
              ig_ = ipm_gen()
              ratio = max(1, int(round(8.0 * (i1 + 1) / 36.0)))
              alive_g, alive_m = True, True
              alive_i = True
              while alive_i:
                  try:
                      next(ig_)
                  except StopIteration:
                      alive_i = False
                  if alive_g:
                      try:
                          next(gg_)
                      except StopIteration:
                          alive_g = False
              while alive_g or alive_m:
                  if alive_g:
                      try:
                          next(gg_)
                      except StopIteration:
                          alive_g = False
                  for _ in range(ratio if alive_g else 4):
                      if alive_m:
                          try:
                              next(mg_)
                          except StopIteration:
                              alive_m = False
              if isdbg:
                  dump("attnT_g", attnT[:, 0:4, :], [("attnT_g", t_) for t_ in range(NT)])
              if isdbg:
                  dump("attnT_m", attnT[:, 4:8, :], [("attnT_m", h_) for h_ in range(8)])
              if stop_after == "moba":
                  break

              ATT_ALL = [("attnT_g", tt) for tt in range(NT)] + [("attnT_m", h) for h in range(8)]
              wvs = [wload(wb_out_v[:, :, ch * 512:(ch + 1) * 512], "wb_out", 8, 512) for ch in range(2)]
              for tt in range(NT):
                  for ch in range(2):
                      wv, wr = wvs[ch]
                      bk = nbank()
                      acc_mm(ps[bk][:, :], [(attnT[:, kc, tt * 128:(tt + 1) * 128], wv[:, kc, :]) for kc in range(8)],
                             r=[wr] + ATT_ALL, w=[("ps", bk)])
                      P.add("dve", lambda e, bk=bk, ch=ch: e.tensor_tensor(out=sig[:], in0=ps[bk][:, :], in1=gate_a[:, ch * 512:(ch + 1) * 512],
                                                                           op=ALU.mult), r=[("ps", bk), ("gate_a", ch)], w=["sig"])
                      P.add("dve", lambda e, tt=tt, ch=ch: e.tensor_tensor(out=xblk[:, tt, ch * 512:(ch + 1) * 512],
                                                                           in0=xblk[:, tt, ch * 512:(ch + 1) * 512], in1=sig[:], op=ALU.add),
                            r=["sig", ("x", tt)], w=[("x", tt)])
              if isdbg:
                  dump("x1", xblk[:].rearrange("p t d -> p (t d)"), [("x", t_) for t_ in range(NT)])
              norm_hT(b, 1)
              for pc in range(8):
                  wv, wr = wload(wb_1_v[:, :, pc * 512:(pc + 1) * 512], "wb_1", 8, 512)
                  for fl in range(4):
                      fc = pc * 4 + fl
                      bk = nbank()
                      acc_mm(ps[bk][:, :], [(wv[:, kc, fl * 128:(fl + 1) * 128], hT[:, kc, :]) for kc in range(8)],
                             r=[wr] + HT_ALL, w=[("ps", bk)])
                      P.add("act", lambda e, bk=bk, fc=fc: e.activation(out=uT[:, fc, :], in_=ps[bk][:, :], func=AF.Relu),
                            r=[("ps", bk)], w=[("uT", fc)])
                      P.add("dve", lambda e, fc=fc: e.tensor_tensor(out=uT[:, fc, :], in0=uT[:, fc, :], in1=uT[:, fc, :], op=ALU.mult),
                            r=[("uT", fc)], w=[("uT", fc)])
              for pc in range(8):
                  wv, wr = wload(wb_2_v[:, pc * 4:(pc + 1) * 4, :], "wb_2", 4, 1024)
                  for fl in range(4):
                      fc = pc * 4 + fl
                      for tt in range(NT):
                          for ch in range(2):
                              bk = tt * 2 + ch
                              P.add("pe", lambda e, bk=bk, fc=fc, fl=fl, tt=tt, ch=ch, wv=wv: e.matmul(
                                  ps[bk][:, :], lhsT=uT[:, fc, tt * 128:(tt + 1) * 128], rhs=wv[:, fl, ch * 512:(ch + 1) * 512],
                                  start=(fc == 0), stop=(fc == 31), skip_group_check=True),
                                  r=[wr, ("uT", fc)], w=[("ps", bk)])
              for tt in range(NT):
                  for ch in range(2):
                      bk = tt * 2 + ch
                      P.add("dve", lambda e, bk=bk, ch=ch: e.tensor_tensor(out=sig[:], in0=ps[bk][:, :], in1=gate_m[:, ch * 512:(ch + 1) * 512],
                                                                           op=ALU.mult), r=[("ps", bk), ("gate_m", ch)], w=["sig"])
                      P.add("dve", lambda e, tt=tt, ch=ch: e.tensor_tensor(out=xblk[:, tt, ch * 512:(ch + 1) * 512],
                                                                           in0=xblk[:, tt, ch * 512:(ch + 1) * 512], in1=sig[:], op=ALU.add),
                            r=["sig", ("x", tt)], w=[("x", tt)])
              norm_stats()
              for tt in range(NT):
                  P.add("dve", lambda e, tt=tt: e.scalar_tensor_tensor(out=xblk[:, tt, :], in0=xblk[:, tt, :], scalar=rstd[:, tt:tt + 1],
                                                                       in1=gfin_bc[:], op0=ALU.mult, op1=ALU.mult),
                        r=[("x", tt), ("rstd", tt), "gfin_bc"], w=[("x", tt)])
                  odst = out_d.ap()[b, T * TB + tt * 128:T * TB + (tt + 1) * 128, :]
                  op = P.add("sp", lambda e, odst=odst, tt=tt: e.dma_start(out=odst, in_=xblk[:, tt, :]), r=[("x", tt)],
                             w=[("out", b, T, tt)], dma_key=("ostore", tt))
                  out_ops.append(op)
          if stop_after is not None:
              break
    except _Stop:
        pass
    if not out_ops:
        op = P.add("sp", lambda e: e.dma_start(out=out_d.ap()[0, 0:TB, :].rearrange("(t p) d -> p t d", p=128), in_=xblk[:]),
                   r=[("x", t_) for t_ in range(NT)], w=[("out", 0, 0)], dma_key="ostore")
        out_ops.append(op)
    fin = [out_ops[-1]]
    for o in P.ops:
        if o.dma_key is not None and isinstance(o.dma_key, tuple) and o.dma_key[0] == "dbg":
            fin.append(o)
    P.finals = fin
    return nc, P


_CACHE = {}


def make_inputs_common(inputs):
    cs = host_consts()
    com = {
        "w_ada": np.ascontiguousarray(inputs["w_ada"][0], dtype=np.float32),
        "b_ada": np.ascontiguousarray(inputs["b_ada"][0].reshape(48, 128), dtype=np.float32),
        "g_mix": np.ascontiguousarray(inputs["g_mix"][0].reshape(8, 128), dtype=np.float32),
        "g_mlp": np.ascontiguousarray(inputs["g_mlp"][0].reshape(8, 128), dtype=np.float32),
        "w_in": np.ascontiguousarray(inputs["w_in"][0], dtype=np.float32),
        "w_gla_gate": np.ascontiguousarray(inputs["w_gla_gate"][0], dtype=np.float32),
        "b_gla_gate": np.ascontiguousarray(inputs["b_gla_gate"][0].reshape(1, 256), dtype=np.float32),
        "g_gla_out": np.ascontiguousarray(inputs["g_gla_out"][0].reshape(1, 512), dtype=np.float32),
        "rel_bias": np.ascontiguousarray(inputs["rel_bias"], dtype=np.float32),
        "w_out": np.ascontiguousarray(inputs["w_out"][0], dtype=np.float32),
        "w_ff1": np.ascontiguousarray(inputs["w_ff1"][0], dtype=np.float32),
        "w_ff2": np.ascontiguousarray(inputs["w_ff2"][0], dtype=np.float32),
        "g_final": np.ascontiguousarray(inputs["g_final"].reshape(1, D), dtype=np.float32),
    }
    com.update(cs)
    return com


def kernel(**inputs):
    x = np.asarray(inputs["x"], dtype=np.float32)
    c = np.asarray(inputs["c"], dtype=np.float32)
    com = make_inputs_common(inputs)
    nc, P = build_nc(SEQ_PER_CORE, NBLK)
    P.emit(nc)
    in_maps = []
    for k in range(NCORES):
        m = dict(com)
        m["x"] = np.ascontiguousarray(x[k * SEQ_PER_CORE:(k + 1) * SEQ_PER_CORE])
        m["c"] = np.ascontiguousarray(c[k * SEQ_PER_CORE:(k + 1) * SEQ_PER_CORE])
        in_maps.append(m)
    res = run_bass_kernel_spmd(nc, in_maps, core_ids=list(range(NCORES)))
    out = np.concatenate([np.asarray(r["out"], dtype=np.float32) for r in res.results], axis=0)
    return out
```

```python
import numpy as np
import concourse.bass as bass
import concourse.mybir as mybir
from concourse.bass_utils import run_bass_kernel_spmd

F32 = mybir.dt.float32
BF16 = mybir.dt.bfloat16
AF = mybir.ActivationFunctionType
ALU = mybir.AluOpType
AX = mybir.AxisListType

D = 1024
S = 2048
NCORES = 8
SEQ_PER_CORE = 4
TB = 512
NT = 4
NBLK = S // TB
DIN = 3088
DFF = 4096
BIG = 30000.0
GBIG = 10000.0
EPS = 1e-6
LT = 768
SAME_ENGINE_SYNC = True


class Op:
    __slots__ = ("idx", "eng", "fn", "deps", "dma_key", "ticket", "signal", "pos")

    def __init__(self, idx, eng, fn, dma_key):
        self.idx = idx
        self.eng = eng
        self.fn = fn
        self.deps = set()
        self.dma_key = dma_key
        self.ticket = None
        self.signal = dma_key is not None
        self.pos = None


class Prog:
    ENGS = ("pe", "act", "dve", "pool", "sp")

    def __init__(self):
        self.ops = []
        self.lastw = {}
        self.rd = {}
        self.finals = []

    def add(self, eng, fn, r=(), w=(), dma_key=None):
        op = Op(len(self.ops), eng, fn, dma_key)
        deps = op.deps
        for res in r:
            o = self.lastw.get(res)
            if o is not None:
                deps.add(o)
            if isinstance(res, tuple) and res[0] == "ps":
                for o in self.rd.get(res, ()):
                    if o.eng != eng:
                        deps.add(o)
        for res in w:
            o = self.lastw.get(res)
            if o is not None:
                deps.add(o)
            for o in self.rd.get(res, ()):
                deps.add(o)
        for res in r:
            self.rd.setdefault(res, []).append(op)
        for res in w:
            self.lastw[res] = op
            self.rd[res] = []
        deps.discard(op)
        self.ops.append(op)
        return op

    def emit(self, nc):
        for op in self.ops:
            for d in op.deps:
                if d.dma_key is None and (d.eng != op.eng or (SAME_ENGINE_SYNC and op.eng != "pe")):
                    d.signal = True
        for op in self.finals:
            op.signal = True
        cnt = {e: 0 for e in self.ENGS}
        dcnt = {}
        for op in self.ops:
            if op.dma_key is not None:
                dcnt[op.dma_key] = dcnt.get(op.dma_key, 0) + 16
                op.ticket = dcnt[op.dma_key]
            elif op.signal:
                cnt[op.eng] += 1
                op.ticket = cnt[op.eng]
        keys = sorted(dcnt.keys(), key=str)
        import contextlib
        with contextlib.ExitStack() as es:
            esem = {e: es.enter_context(nc.semaphore("e_" + e)) for e in self.ENGS}
            dsem = {k: es.enter_context(nc.semaphore("d_" + str(i))) for i, k in enumerate(keys)}
            block = es.enter_context(nc.Block())
            by_eng = {e: [op for op in self.ops if op.eng == e] for e in self.ENGS}

            def run(engname, e):
                known = {}
                for op in by_eng[engname]:
                    waits = {}
                    for d in op.deps:
                        if d.dma_key is not None:
                            sem = dsem[d.dma_key]
                        else:
                            if d.eng == op.eng and (not SAME_ENGINE_SYNC or op.eng == "pe"):
                                continue
                            sem = esem[d.eng]
                        k = id(sem)
                        if d.ticket > waits.get(k, (None, 0))[1]:
                            waits[k] = (sem, d.ticket)
                    for k, (sem, v) in waits.items():
                        if known.get(k, 0) >= v:
                            continue
                        known[k] = v
                        e.wait_ge(sem, v)
                    ins = op.fn(e)
                    if op.dma_key is not None:
                        ins.then_inc(dsem[op.dma_key], 16)
                    elif op.signal:
                        ins.then_inc(esem[op.eng], 1)
                if engname == "sp":
                    for k in keys:
                        e.wait_ge(dsem[k], dcnt[k])

            @block.tensor
            def _(e):
                run("pe", e)

            @block.scalar
            def _(e):
                run("act", e)

            @block.vector
            def _(e):
                run("dve", e)

            @block.gpsimd
            def _(e):
                run("pool", e)

            @block.sync
            def _(e):
                run("sp", e)


def _t5_bucket(rel):
    n = np.maximum(rel, 0)
    max_exact = 16
    ratio = np.maximum(n, 1).astype(np.float32) / np.float32(max_exact)
    large = max_exact + (np.log(ratio).astype(np.float32) / np.float32(np.log(128 / 16)) * np.float32(16)).astype(np.int32)
    large = np.minimum(large, 31)
    return np.where(n < max_exact, n, large)


def host_consts():
    cf = np.zeros((128, 1032), np.float32)
    cf[:, 0:128] = np.eye(128)
    cf[:, 128:256] = 1.0
    j = np.arange(128)[:, None]
    i = np.arange(128)[None, :]
    cf[:, 256:384] = np.where(j <= i, -1.0 / 16, 0.0)
    cf[:, 384:512] = np.where(j > i, -1.0 / 16, 0.0)
    gm = np.zeros((8, 8, 8), np.float32)
    for own in range(8):
        for n in range(8):
            gm[own, :, n] = GBIG if n == own else (-GBIG if n > own else 0.0)
    cf[:, 512:1024] = gm.reshape(1, 512)
    for h in range(8):
        cf[h, 1024 + h] = 1.0
    cb = np.zeros((128, 288), np.float32)
    cb[:, 0:128] = np.eye(128)
    cb[:, 128:256] = np.where(j <= i, 1.0, 0.0)
    for pr in range(4):
        cb[0:64, 256 + pr * 8 + 2 * pr] = 1.0
        cb[64:128, 256 + pr * 8 + 2 * pr + 1] = 1.0
    ohk = np.zeros((8, 8 * 128), np.float32)
    for jj in range(8):
        ohk[jj, jj * 128:(jj + 1) * 128] = 1.0
    ehot = np.zeros((33, LT), np.float32)
    u = np.arange(LT)
    rel = u - 255
    bk = _t5_bucket(rel)
    for uu in range(LT):
        if rel[uu] >= 0:
            ehot[bk[uu], uu] += 1.0
        else:
            ehot[32, uu] = -BIG
    ehot[31, :] -= 1.0
    return {"cf": cf, "cb": cb, "ohk": ohk, "ehot": ehot}


def build_nc(nseq=SEQ_PER_CORE, nblk=NBLK, dbg=None, stop_after=None):
    nc = bass.Bass("TRN2", target_bir_lowering=False)
    P = Prog()
    dbg = dbg or {}

    def din(name, shape):
        return nc.dram_tensor(name, list(shape), F32, kind="ExternalInput")

    x_d = din("x", (nseq, S, D))
    c_d = din("c", (4, D))
    w_ada_d = din("w_ada", (D, 6 * D))
    b_ada_d = din("b_ada", (48, 128))
    g_mix_d = din("g_mix", (8, 128))
    g_mlp_d = din("g_mlp", (8, 128))
    w_in_d = din("w_in", (D, DIN))
    w_gg_d = din("w_gla_gate", (16, 256))
    b_gg_d = din("b_gla_gate", (1, 256))
    g_out_d = din("g_gla_out", (1, 512))
    relb_d = din("rel_bias", (32, 8))
    w_out_d = din("w_out", (D, D))
    w_ff1_d = din("w_ff1", (D, DFF))
    w_ff2_d = din("w_ff2", (DFF, D))
    g_fin_d = din("g_final", (1, D))
    cf_d = din("cf", (128, 1032))
    cb_d = din("cb", (128, 288))
    ohk_d = din("ohk", (8, 1024))
    ehot_d = din("ehot", (33, LT))
    out_d = nc.dram_tensor("out", [nseq, S, D], F32, kind="ExternalOutput")
    dbg_t = {k: nc.dram_tensor("dbg_" + k, list(shp), F32, kind="ExternalOutput") for k, shp in dbg.items()}

    wb_in = nc.dram_tensor("wb_in", [D, DIN], BF16)
    wb_out = nc.dram_tensor("wb_out", [D, D], BF16)
    wb_1 = nc.dram_tensor("wb_1", [D, DFF], BF16)
    wb_2 = nc.dram_tensor("wb_2", [DFF, D], BF16)
    TOEPN = 128 * LT + 1024
    toep = nc.dram_tensor("toep", [8 * TOEPN], F32)

    def sb(name, shape, dt):
        return nc.alloc_sbuf_tensor("s_" + name, list(shape), dt)

    def A(t, off, dims):
        return bass.AP(t, off, [list(d) for d in dims])

    cf = sb("cf", (128, 1032), F32)
    cb = sb("cb", (128, 288), BF16)
    ohk = sb("ohk", (128, 1024), BF16)
    wslot = [sb("wslot%d" % i, (128, 4096), BF16) for i in range(2)]
    xblk = sb("xblk", (128, NT, D), F32)
    hT = sb("hT", (128, 8, TB), BF16)
    gqT = sb("gqT", (128, 2, TB), F32)
    gkT = sb("gkT", (128, 2, TB), F32)
    gktok = sb("gktok", (128, NT, 256), F32)
    gv = sb("gv", (128, NT, 512), BF16)
    t3 = sb("t3", (128, NT, 512), BF16)
    sig = sb("sig", (128, 512), F32)
    ggT = sb("ggT", (17, TB), BF16)
    wg = sb("wg", (17, 256), BF16)
    Qt = sb("Qt", (128, 4, TB), BF16)
    Kt = sb("Kt", (128, 4, S), BF16)
    Vint = sb("Vint", (128, 16, 768), BF16)
    maskT = [sb("maskT%d" % i, (128, TB), BF16) for i in range(2)]
    attnT = sb("attnT", (128, 8, TB), BF16)
    arena = sb("arena", (128, 8192), F32)
    gate_a = sb("gate_a", (128, D), F32)
    gate_m = sb("gate_m", (128, D), F32)
    gout_bc = sb("gout_bc", (128, 512), F32)
    gfin_bc = sb("gfin_bc", (128, D), F32)
    Mown = sb("Mown", (128, 8, 256), BF16)
    Mprev = sb("Mprev", (128, 8, 112), BF16)
    b31bc = sb("b31bc", (128, 8), F32)
    Lg = sb("Lg", (128, 256), F32)
    eA = sb("eA", (128, 2, 128), F32)
    eK = sb("eK", (128, 2, 128), F32)
    eB = sb("eB", (128, 2, 128), F32)
    eC = sb("eC", (128, 256), F32)
    qAz = [sb("qAz%d" % i, (128, 2, 128), BF16) for i in range(2)]
    kA = sb("kA", (128, 2, 128), BF16)
    qBz2 = [[sb("qBz%d_%d" % (j, i), (128, 2, 128), BF16) for i in range(2)] for j in range(2)]
    kC2 = [sb("kC%d" % i, (128, 256), BF16) for i in range(2)]
    attT2 = [sb("attT%d" % i, (128, 4, 128), BF16) for i in range(2)]
    on_ = sb("on", (128, 512), BF16)
    Sf = sb("Sf", (128, 2, 128), F32)
    Sb = sb("Sb", (128, 2, 128), BF16)
    brf = sb("brf", (128, 2), F32)
    nbrf = sb("nbrf", (128, 2), F32)
    dec2 = [sb("dec%d" % i, (128, 2), F32) for i in range(2)]
    oms = sb("oms", (128, 4), F32)
    orst = sb("orst", (128, 4), F32)
    ms = sb("ms", (128, NT), F32)
    rstd = sb("rstd", (128, NT), F32)
    NPT = 6
    Pt = [sb("Pt%d" % i, (128, TB), BF16) for i in range(NPT)]
    gmx = sb("gmx", (128, 64), F32)
    m8 = sb("m8", (128, 8, 8), F32)
    selt = sb("selt", (128, 64), F32)
    mrow = sb("mrow", (128, NT, 64), BF16)
    kmsum = sb("kmsum", (128, 4, 8), F32)
    kmean = sb("kmean", (128, 4, 8), BF16)
    qn2 = sb("qn2", (8, 1), F32)
    kn2 = sb("kn2", (8, 1), F32)
    kn2r = sb("kn2r", (8, 1), F32)
    bnd = sb("bnd", (8, 1), F32)
    bnd8 = sb("bnd8", (8, 8), F32)
    nbias = sb("nbias", (128, 8), F32)
    cactT = sb("cactT", (128, 32), F32)
    b48 = sb("b48", (48, 128), F32)
    g8 = sb("g8", (8, 2, 128), F32)
    badaT = sb("badaT", (128, 48), F32)
    gT = sb("gT", (128, 2, 8), F32)
    adaT = sb("adaT", (128, 48, 4), F32)
    G1T = sb("G1T", (128, 8, 4), F32)
    G2T = sb("G2T", (128, 8, 4), F32)
    diag = [sb("diag%d" % i, (128, 128), F32) for i in range(2)]
    rb33 = sb("rb33", (33, 8), F32)
    rbx = sb("rbx", (33, 128), F32)

    ab = arena[:].bitcast(BF16)
    uT = ab.rearrange("p (c t) -> p c t", t=TB)
    sq = ab[:, 0:4 * TB].rearrange("p (a t) -> p a t", t=TB)
    xsfull = ab[:, 8 * TB:16 * TB].rearrange("p (t d) -> p t d", d=D)
    junk = ab[:, 30 * TB:32 * TB]
    wst = arena[:, 0:4096].rearrange("p (s k n) -> p s k n", s=2, k=8)
    Gtabs = [arena[:, 4096:4096 + LT], arena[:, 4864:4864 + LT]]
    ehot_sb = arena[0:33, 5632:5632 + LT]
    c4 = arena[0:4, 6400:7424]
    c4e = gate_m[0:4, :]
    rden2 = [sb("rdenA", (128, TB), F32), sb("rdenB", (128, TB), F32)]
    XS_RES = [[("uT", 8 + 2 * t_), ("uT", 9 + 2 * t_)] for t_ in range(NT)]
    JUNK_RES = [("uT", 30), ("uT", 31)]
    SQ_RES = [("uT", c_) for c_ in range(4)]

    NB_RING = 6
    ps = [nc.alloc_psum_tensor("ps%d" % i, [128, 512], F32) for i in range(8)]
    class Ring:
        def __init__(self, banks):
            self.banks = list(banks)
            self.i = 0

        def reset(self):
            self.i = 0

    ringD = Ring(range(6))
    ringG = Ring([0])
    ringB = Ring([1, 2])
    ringM = Ring([2, 3, 4, 5])

    def nbank(ring=None):
        ring = ring or ringD
        k = ring.banks[ring.i % len(ring.banks)]
        ring.i += 1
        return k

    ident_f = cf[:, 0:128]
    ones_f = cf[:, 128:256]
    ident_b = cb[:, 0:128]

    def dump(name, src_ap, res):
        if name in dbg_t:
            t = dbg_t[name]
            P.add("pool", lambda e: e.dma_start(out=t.ap(), in_=src_ap), r=res, w=[("dbg", name)], dma_key=("dbg", name))

    P.add("sp", lambda e: e.dma_start(out=cf[:], in_=cf_d.ap()[:, :]), w=["cf"], dma_key="cf")
    P.add("pool", lambda e: e.dma_start(out=cb[:], in_=cb_d.ap()[:, :]), w=["cb"], dma_key="cb")
    P.add("pool", lambda e: e.dma_start(out=ohk[0:8, :], in_=ohk_d.ap()[:, :]), w=["ohk"], dma_key="ohk")
    P.add("pool", lambda e: e.dma_start(out=ohk[64:72, :], in_=ohk_d.ap()[:, :]), w=["ohk2"], dma_key="ohk2")
    P.add("pool", lambda e: e.dma_start(out=wg[0:16, :], in_=w_gg_d.ap()[:, :]), w=["wg0"], dma_key="wg0")
    P.add("pool", lambda e: e.dma_start(out=wg[16:17, :], in_=b_gg_d.ap()[:, :]), w=["wg1"], dma_key="wg1")
    WB_RES = {}
    for (nm, dst, src, rows) in (("wb_in", wb_in, w_in_d, D), ("wb_out", wb_out, w_out_d, D),
                                 ("wb_1", wb_1, w_ff1_d, D), ("wb_2", wb_2, w_ff2_d, DFF)):
        nsp = 4
        rr = rows // nsp
        WB_RES[nm] = []
        for q in range(nsp):
            key = (nm, q)
            P.add("pool", lambda e, dst=dst, src=src, q=q, rr=rr: e.dma_start(
                out=dst.ap()[q * rr:(q + 1) * rr, :], in_=src.ap()[q * rr:(q + 1) * rr, :]),
                w=[key], dma_key=key)
            WB_RES[nm].append(key)

    P.add("sp", lambda e: e.dma_start(out=gout_bc[:], in_=A(g_out_d, 0, [[0, 128], [1, 512]])), w=["gout_bc"], dma_key="gout")
    P.add("sp", lambda e: e.dma_start(out=gfin_bc[:], in_=A(g_fin_d, 0, [[0, 128], [1, D]])), w=["gfin_bc"], dma_key="gfin")
    P.add("sp", lambda e: e.dma_start(out=b31bc[:], in_=A(relb_d, 31 * 8, [[0, 128], [1, 8]])), w=["b31bc"], dma_key="b31")
    P.add("sp", lambda e: e.dma_start(out=rb33[0:32, :], in_=relb_d.ap()[:, :]), w=["rb33a"], dma_key="rb33")
    P.add("sp", lambda e: e.dma_start(out=c4, in_=c_d.ap()[:, :]), w=["c4"], dma_key="c4")
    P.add("sp", lambda e: e.dma_start(out=b48[:], in_=b_ada_d.ap()[:, :]), w=["b48"], dma_key="b48")
    P.add("sp", lambda e: e.dma_start(out=g8[:, 0, :], in_=g_mix_d.ap()[:, :]), w=["g8a"], dma_key="g8a")
    P.add("sp", lambda e: e.dma_start(out=g8[:, 1, :], in_=g_mlp_d.ap()[:, :]), w=["g8b"], dma_key="g8b")
    P.add("sp", lambda e: e.dma_start(out=ehot_sb, in_=ehot_d.ap()[:, :]), w=["ehot"], dma_key="ehot")

    P.add("dve", lambda e: e.memset(ggT[:], 1.0), w=["ggT"])
    P.add("dve", lambda e: e.memset(Vint[:], 1.0), w=[("V", t_) for t_ in range(16)])
    P.add("dve", lambda e: e.memset(kmsum[:], 0.0), w=["kmsum"])
    for i_ in range(2):
        P.add("dve", lambda e, i_=i_: e.memset(qAz[i_][:], 0.0), w=[("qA", i_)])
        for j_ in range(2):
            P.add("dve", lambda e, i_=i_, j_=j_: e.memset(qBz2[j_][i_][:], 0.0), w=[("qB", j_, i_)])
    P.add("dve", lambda e: e.memset(kmean[:], 0.0), w=["kmean"])
    P.add("dve", lambda e: e.memset(rb33[32:33, :], 1.0), w=["rb33b"])

    P.add("act", lambda e: e.activation(out=c4e, in_=c4, func=AF.Exp, scale=-1.0), r=["c4"], w=["c4e"])
    P.add("dve", lambda e: e.tensor_scalar(out=c4e, in0=c4e, scalar1=1.0, scalar2=None, op0=ALU.add), r=["c4e"], w=["c4e"])
    P.add("dve", lambda e: e.reciprocal(out=c4e, in_=c4e), r=["c4e"], w=["c4e"])
    P.add("dve", lambda e: e.tensor_tensor(out=c4, in0=c4, in1=c4e, op=ALU.mult), r=["c4e", "c4"], w=["c4"])
    bk = nbank()
    for kc in range(8):
        P.add("pe", lambda e, kc=kc, bk=bk: e.matmul(ps[bk][:, kc * 4:kc * 4 + 4], lhsT=c4[:, kc * 128:(kc + 1) * 128],
                                                     rhs=cf[0:4, 0:4], start=True, stop=True),
              r=["c4", "cf"], w=[("ps", bk)])
    P.add("dve", lambda e, bk=bk: e.tensor_copy(out=cactT[:], in_=ps[bk][:, 0:32]), r=[("ps", bk)], w=["cactT"])
    bk = nbank()
    P.add("pe", lambda e, bk=bk: e.matmul(ps[bk][:, 0:48], lhsT=b48[0:48, :], rhs=cf[0:48, 0:48], start=True, stop=True),
          r=["b48", "cf"], w=[("ps", bk)])
    for q in range(2):
        P.add("pe", lambda e, bk=bk, q=q: e.matmul(ps[bk][:, 64 + q * 8:72 + q * 8], lhsT=g8[0:8, q, :], rhs=cf[0:8, 0:8],
                                                   start=True, stop=True),
              r=["g8a", "g8b", "cf"], w=[("ps", bk)])
    P.add("dve", lambda e, bk=bk: e.tensor_copy(out=badaT[:], in_=ps[bk][:, 0:48]), r=[("ps", bk)], w=["badaT"])
    P.add("dve", lambda e, bk=bk: e.tensor_copy(out=gT[:], in_=ps[bk][:, 64:80].rearrange("p (q c) -> p q c", c=8)),
          r=[("ps", bk)], w=["gT"])
    bk_ada = nbank()
    w_ada_v = w_ada_d.ap().rearrange("(k p) n -> p k n", p=128)
    for pc in range(24):
        sl = pc % 2
        P.add("sp", lambda e, pc=pc, sl=sl: e.dma_start(out=wst[:, sl], in_=w_ada_v[:, :, pc * 256:(pc + 1) * 256]),
              w=[("wst", sl)], dma_key=("wst", sl))
        for sub in range(2):
            blk = pc * 2 + sub
            for kc in range(8):
                P.add("pe", lambda e, sl=sl, sub=sub, kc=kc, blk=blk: e.matmul(
                    ps[bk_ada][:, blk * 4:blk * 4 + 4], lhsT=wst[:, sl, kc, sub * 128:(sub + 1) * 128],
                    rhs=cactT[:, kc * 4:kc * 4 + 4], start=(kc == 0), stop=(kc == 7)),
                    r=[("wst", sl), "cactT"], w=[("ps", bk_ada)])
    P.add("dve", lambda e: e.tensor_tensor(out=adaT[:], in0=ps[bk_ada][:, 0:192].rearrange("p (k b) -> p k b", b=4),
                                           in1=A(badaT, 0, [[48, 128], [1, 48], [0, 4]]), op=ALU.add),
          r=[("ps", bk_ada), "badaT"], w=["adaT"])
    for (GT, sc0, q) in ((G1T, 8, 0), (G2T, 32, 1)):
        P.add("dve", lambda e, GT=GT, sc0=sc0: e.tensor_scalar(out=GT[:], in0=adaT[:, sc0:sc0 + 8, :], scalar1=1.0,
                                                               scalar2=None, op0=ALU.add), r=["adaT"], w=[("G", q)])
        P.add("dve", lambda e, GT=GT, q=q: e.tensor_tensor(out=GT[:], in0=GT[:], in1=A(gT, q * 8, [[16, 128], [1, 8], [0, 4]]),
                                                           op=ALU.mult), r=[("G", q), "gT"], w=[("G", q)])
    dump("adaT", adaT[:].rearrange("p k b -> p (k b)"), ["adaT"])

    for h in range(8 if stop_after != "pro_a" else 0):
        Gtab = Gtabs[h % 2]
        gres = ("Gtab", h % 2)
        P.add("dve", lambda e, h=h: e.tensor_scalar(out=rbx[:], in0=cf[0:33, 128:256], scalar1=rb33[0:33, h:h + 1],
                                                    scalar2=None, op0=ALU.mult),
              r=["cf", "rb33a", "rb33b"], w=["rbx"])
        for half in range(2):
            b_ = nbank()
            P.add("pe", lambda e, half=half, b_=b_: e.matmul(ps[b_][:, 0:384], lhsT=rbx[0:33, :],
                                                             rhs=ehot_sb[:, half * 384:(half + 1) * 384], start=True, stop=True),
                  r=["rbx", "ehot"], w=[("ps", b_)])
            P.add("act", lambda e, half=half, b_=b_, Gtab=Gtab: e.activation(out=Gtab[:, half * 384:(half + 1) * 384],
                                                                            in_=ps[b_][:, 0:384], func=AF.Exp),
                  r=[("ps", b_)], w=[gres])
        P.add("sp", lambda e, h=h, Gtab=Gtab: e.dma_start(out=A(toep, h * TOEPN, [[LT, 128], [1, LT]]), in_=Gtab),
              r=[gres], w=[("toep", h)], dma_key=("toepw", h % 2))
        P.add("pool", lambda e, h=h: e.dma_start(out=Mown[:, h, :], in_=A(toep, h * TOEPN + 255, [[LT - 1, 128], [1, 256]])),
              r=[("toep", h)], w=[("Mown", h)], dma_key=("toepr", h))
        P.add("pool", lambda e, h=h: e.dma_start(out=Mprev[:, h, :], in_=A(toep, h * TOEPN + 383, [[LT - 1, 128], [1, 112]])),
              r=[("toep", h)], w=[("Mprev", h)], dma_key=("toepr2", h))

    wctr = [0]

    def wload(src_ap, wb_name, k, n):
        sl = wctr[0] % 2
        wctr[0] += 1
        view = wslot[sl][:, 0:k * n].rearrange("p (k n) -> p k n", n=n)
        res = ("wslot", sl)
        P.add("sp", lambda e: e.dma_start(out=view, in_=src_ap), r=WB_RES[wb_name], w=[res], dma_key=res)
        return view, res

    wb_in_v = wb_in.ap().rearrange("(k p) n -> p k n", p=128)
    wb_out_v = wb_out.ap().rearrange("(k p) n -> p k n", p=128)
    wb_1_v = wb_1.ap().rearrange("(k p) n -> p k n", p=128)
    wb_2_v = wb_2.ap().rearrange("(k p) n -> p k n", p=128)

    def norm_stats():
        for tt in range(NT):
            P.add("act", lambda e, tt=tt: e.activation(out=junk, in_=xblk[:, tt, :], func=AF.Square, scale=1.0 / 32.0,
                                                       accum_out=ms[:, tt:tt + 1]),
                  r=[("x", tt)], w=JUNK_RES + [("ms", tt)])
            P.add("act", lambda e, tt=tt: e.activation(out=rstd[:, tt:tt + 1], in_=ms[:, tt:tt + 1], func=AF.Ln, bias=EPS, scale=1.0),
                  r=[("ms", tt)], w=[("rstd", tt)])
            P.add("act", lambda e, tt=tt: e.activation(out=rstd[:, tt:tt + 1], in_=rstd[:, tt:tt + 1], func=AF.Exp, scale=-0.5),
                  r=[("rstd", tt)], w=[("rstd", tt)])

    def norm_hT(b, which):
        GT = G1T if which == 0 else G2T
        gq_ = ("G", which)
        sh0 = 0 if which == 0 else 24
        norm_stats()
        for half in range(2):
            banks = [nbank() for _ in range(4)]
            for tt in range(NT):
                if half == 0:
                    P.add("dve", lambda e, tt=tt: e.tensor_scalar(out=xsfull[:, tt, :], in0=xblk[:, tt, :],
                                                                  scalar1=rstd[:, tt:tt + 1], scalar2=None, op0=ALU.mult),
                          r=[("x", tt), ("rstd", tt), "adaT"], w=XS_RES[tt])
                for q in range(4):
                    kc = half * 4 + q
                    P.add("pe", lambda e, tt=tt, kc=kc, bq=banks[q]: e.matmul(
                        ps[bq][:, tt * 128:(tt + 1) * 128], lhsT=xsfull[:, tt, kc * 128:(kc + 1) * 128], rhs=ident_b,
                        start=True, stop=True), r=XS_RES[tt] + ["cb"], w=[("ps", banks[q])])
            for q in range(4):
                kc = half * 4 + q
                bq = banks[q]
                if q % 2 == 0:
                    P.add("act", lambda e, kc=kc, bq=bq: e.activation(out=hT[:, kc, :], in_=ps[bq][:, :], func=AF.Identity,
                                                                     scale=GT[:, kc, b:b + 1], bias=adaT[:, sh0 + kc, b:b + 1]),
                          r=[("ps", bq), gq_, "adaT"], w=[("hT", kc)])
                else:
                    P.add("dve", lambda e, kc=kc, bq=bq: e.tensor_scalar(out=hT[:, kc, :], in0=ps[bq][:, :],
                                                                        scalar1=GT[:, kc, b:b + 1], scalar2=adaT[:, sh0 + kc, b:b + 1],
                                                                        op0=ALU.mult, op1=ALU.add),
                          r=[("ps", bq), gq_, "adaT"], w=[("hT", kc)])

    def acc_mm(out_ap, pairs, r, w):
        n = len(pairs)
        for i_, (l, rh) in enumerate(pairs):
            P.add("pe", lambda e, l=l, rh=rh, i_=i_: e.matmul(out_ap, lhsT=l, rhs=rh, start=(i_ == 0), stop=(i_ == n - 1)),
                  r=r, w=w)

    HT_ALL = [("hT", kc) for kc in range(8)]
    first = [True]

    class _Stop(Exception):
        pass

    def chk(name):
        if stop_after == name:
            raise _Stop()

    out_ops = []
    try:
      for b in range(nseq):
          if stop_after in ("prologue", "pro_a", "pro_b"):
              break
          for (gt_, blk0, nm) in ((gate_a, 16, "gate_a"), (gate_m, 40, "gate_m")):
              for half in range(2):
                  bk = nbank()
                  for q in range(4):
                      kc = half * 4 + q
                      dg = diag[kc % 2]
                      P.add("dve", lambda e, dg=dg, kc=kc, blk0=blk0, b=b: e.tensor_scalar(out=dg[:], in0=ident_f,
                                                                                     scalar1=adaT[:, blk0 + kc, b:b + 1], scalar2=None,
                                                                                     op0=ALU.mult),
                            r=["cf", "adaT"], w=[("diag", kc % 2)])
                      P.add("pe", lambda e, dg=dg, q=q, bk=bk: e.matmul(ps[bk][:, q * 128:(q + 1) * 128], lhsT=ones_f, rhs=dg[:],
                                                                        start=True, stop=True),
                            r=[("diag", kc % 2), "cf"], w=[("ps", bk)])
                  P.add("act", lambda e, gt_=gt_, half=half, bk=bk: e.activation(out=gt_[:, half * 512:(half + 1) * 512],
                                                                                in_=ps[bk][:, :], func=AF.Copy),
                        r=[("ps", bk), "cactT"], w=[(nm, half)])
          P.add("dve", lambda e: e.memset(Sf[:], 0.0), w=["Sf"])
          P.add("dve", lambda e: e.memset(Sb[:], 0.0), w=["Sb"])
          P.add("dve", lambda e: e.memset(kn2r[:], 0.0), w=["kn2r"])

          for T in range(nblk):
              i0, i1 = 2 * T, 2 * T + 1
              isdbg = first[0]
              first[0] = False
              for tt in range(NT):
                  xsrc = x_d.ap()[b, T * TB + tt * 128:T * TB + (tt + 1) * 128, :]
                  P.add("sp", lambda e, xsrc=xsrc, tt=tt: e.dma_start(out=xblk[:, tt, :], in_=xsrc), w=[("x", tt)],
                        dma_key=("xload", tt))
              norm_hT(b, 0)
              if isdbg:
                  dump("hT", hT[:].rearrange("p k t -> p (k t)"), HT_ALL)
              if stop_after == "norm":
                  break
              wv, wr = wload(wb_in_v[:, :, 0:512], "wb_in", 8, 512)
              for cbk in range(4):
                  bk = nbank()
                  acc_mm(ps[bk][:, :], [(wv[:, kc, cbk * 128:(cbk + 1) * 128], hT[:, kc, :]) for kc in range(8)],
                         r=[wr] + HT_ALL, w=[("ps", bk)])
                  if cbk < 2:
                      P.add("act", lambda e, cbk=cbk, bk=bk: e.activation(out=gqT[:, cbk, :], in_=ps[bk][:, :], func=AF.Identity, scale=0.125),
                            r=[("ps", bk)], w=[("gqT", cbk)])
                  else:
                      P.add("dve", lambda e, cbk=cbk, bk=bk: e.tensor_copy(out=gkT[:, cbk - 2, :], in_=ps[bk][:, :]),
                            r=[("ps", bk)], w=[("gkT", cbk - 2)])
              for tt in range(NT):
                  bk = nbank()
                  acc_mm(ps[bk][:, 0:256], [(hT[:, kc, tt * 128:(tt + 1) * 128], wv[:, kc, 256:512]) for kc in range(8)],
                         r=[wr] + HT_ALL, w=[("ps", bk)])
                  P.add("act", lambda e, tt=tt, bk=bk: e.activation(out=gktok[:, tt, :], in_=ps[bk][:, 0:256], func=AF.Copy),
                        r=[("ps", bk)], w=[("gktok", tt)])
              chk("ip0")
              wv, wr = wload(wb_in_v[:, :, 512:1024], "wb_in", 8, 512)
              for tt in range(NT):
                  bk = nbank()
                  acc_mm(ps[bk][:, :], [(hT[:, kc, tt * 128:(tt + 1) * 128], wv[:, kc, :]) for kc in range(8)],
                         r=[wr] + HT_ALL, w=[("ps", bk)])
                  P.add("dve", lambda e, tt=tt, bk=bk: e.tensor_copy(out=gv[:, tt, :], in_=ps[bk][:, :]),
                        r=[("ps", bk)], w=[("gv", tt)])
              chk("ip1")
              wv, wr = wload(wb_in_v[:, :, 1024:1536], "wb_in", 8, 512)
              for tt in range(NT):
                  bk = nbank()
                  acc_mm(ps[bk][:, :], [(hT[:, kc, tt * 128:(tt + 1) * 128], wv[:, kc, :]) for kc in range(8)],
                         r=[wr] + HT_ALL, w=[("ps", bk)])
                  P.add("act", lambda e, bk=bk: e.activation(out=sig[:], in_=ps[bk][:, :], func=AF.Exp, scale=-1.0),
                        r=[("ps", bk)], w=["sig"])
                  P.add("dve", lambda e, tt=tt, bk=bk: e.tensor_tensor(out=t3[:, tt, :], in0=ps[bk][:, :], in1=gout_bc[:], op=ALU.mult),
                        r=[("ps", bk), "gout_bc"], w=[("t3", tt)])
                  P.add("act", lambda e: e.activation(out=sig[:], in_=sig[:], func=AF.Ln, bias=1.0, scale=1.0), r=["sig"], w=["sig"])
                  P.add("act", lambda e: e.activation(out=sig[:], in_=sig[:], func=AF.Exp, scale=-1.0), r=["sig"], w=["sig"])
                  P.add("dve", lambda e, tt=tt: e.tensor_tensor(out=t3[:, tt, :], in0=t3[:, tt, :], in1=sig[:], op=ALU.mult),
                        r=["sig", ("t3", tt)], w=[("t3", tt)])
              chk("ip2")
              wvg, wrg = wload(wb_in_v[:, :, 1536:1552], "wb_in", 8, 16)
              bk = nbank()
              acc_mm(ps[bk][0:16, :], [(wvg[:, kc, 0:16], hT[:, kc, :]) for kc in range(8)], r=[wrg] + HT_ALL, w=[("ps", bk)])
              P.add("act", lambda e, bk=bk: e.activation(out=ggT[0:16, :], in_=ps[bk][0:16, :], func=AF.Copy),
                    r=[("ps", bk)], w=["ggT"])
              chk("ip3")
              wv, wr = wload(wb_in_v[:, :, 1552:2064], "wb_in", 8, 512)
              for pr in range(4):
                  bk = nbank()
                  acc_mm(ps[bk][:, :], [(wv[:, kc, pr * 128:(pr + 1) * 128], hT[:, kc, :]) for kc in range(8)],
                         r=[wr] + HT_ALL, w=[("ps", bk)])
                  P.add("act", lambda e, pr=pr, bk=bk: e.activation(out=Qt[:, pr, :], in_=ps[bk][:, :], func=AF.Identity, scale=0.125),
                        r=[("ps", bk)], w=[("Qt", pr)])
              chk("ip4")
              wv, wr = wload(wb_in_v[:, :, 2064:2576], "wb_in", 8, 512)
              for pr in range(4):
                  bk = nbank()
                  acc_mm(ps[bk][:, :], [(wv[:, kc, pr * 128:(pr + 1) * 128], hT[:, kc, :]) for kc in range(8)],
                         r=[wr] + HT_ALL, w=[("ps", bk)])
                  P.add("dve", lambda e, pr=pr, bk=bk, T=T: e.tensor_copy(out=Kt[:, pr, T * TB:(T + 1) * TB], in_=ps[bk][:, :]),
                        r=[("ps", bk)], w=[("Kt", pr, T)])
              chk("ip5")
              wv, wr = wload(wb_in_v[:, :, 2576:3088], "wb_in", 8, 512)
              for tt in range(NT):
                  g_t = T * NT + tt
                  bk = nbank()
                  acc_mm(ps[bk][:, :], [(hT[:, kc, tt * 128:(tt + 1) * 128], wv[:, kc, :]) for kc in range(8)],
                         r=[wr] + HT_ALL, w=[("ps", bk)])
                  P.add("act", lambda e, g_t=g_t, bk=bk: e.activation(
                      out=A(Vint, g_t * 768, [[16 * 768, 128], [192, 4], [1, 64]]),
                      in_=A(ps[bk], 0, [[512, 128], [128, 4], [1, 64]]), func=AF.Copy),
                      r=[("ps", bk)], w=[("V", g_t)])
                  P.add("dve", lambda e, g_t=g_t, bk=bk: e.tensor_copy(
                      out=A(Vint, g_t * 768 + 128, [[16 * 768, 128], [192, 4], [1, 64]]),
                      in_=A(ps[bk], 64, [[512, 128], [128, 4], [1, 64]])),
                      r=[("ps", bk), ("V", g_t)], w=[("V", g_t)])
              if isdbg:
                  dump("gqT", gqT[:].rearrange("p k t -> p (k t)"), [("gqT", 0), ("gqT", 1)])
                  dump("gktok", gktok[:].rearrange("p k t -> p (k t)"), [("gktok", t_) for t_ in range(NT)])
                  dump("t3", t3[:].rearrange("p k t -> p (k t)"), [("t3", t_) for t_ in range(NT)])
                  dump("Qt", Qt[:].rearrange("p k t -> p (k t)"), [("Qt", p_) for p_ in range(4)])
                  dump("Kt", Kt[:, :, 0:TB], [("Kt", p_, 0) for p_ in range(4)])
                  dump("Vint", Vint[:, 0:4, :].rearrange("p k t -> p (k t)"), [("V", t_) for t_ in range(4)])
              if stop_after == "inproj":
                  break

              def gla_gen():
                  def gla_front(tt):
                      csl = slice(tt * 128, (tt + 1) * 128)
                      bz = nbank(ringG)
                      P.add("pe", lambda e, bz=bz, csl=csl: e.matmul(ps[bz][:, 0:256], lhsT=ggT[0:17, csl], rhs=wg[0:17, :], start=True, stop=True),
                            r=["ggT", "wg0", "wg1"], w=[("ps", bz)])
                      P.add("act", lambda e, bz=bz: e.activation(out=Lg[:], in_=ps[bz][:, 0:256], func=AF.Exp, scale=-1.0),
                            r=[("ps", bz)], w=["Lg"])
                      P.add("act", lambda e: e.activation(out=Lg[:], in_=Lg[:], func=AF.Ln, bias=1.0, scale=1.0), r=["Lg"], w=["Lg"])
                      yield
                      bc_ = nbank(ringG)
                      bt_ = bc_
                      P.add("pe", lambda e, bc_=bc_: e.matmul(ps[bc_][:, 0:256], lhsT=cf[:, 384:512], rhs=Lg[:], start=True, stop=True),
                            r=["Lg", "cf"], w=[("ps", bc_)])
                      for blk in range(2):
                          P.add("pe", lambda e, bt_=bt_, blk=blk: e.matmul(ps[bt_][:, 256 + blk * 128:256 + (blk + 1) * 128],
                                                                           lhsT=Lg[:, blk * 128:(blk + 1) * 128], rhs=cf[:, 256:384],
                                                                           start=True, stop=True),
                                r=["Lg", "cf"], w=[("ps", bt_)])
                      bT3 = ps[bt_][:, 256:512].rearrange("p (k t) -> p k t", t=128)
                      P.add("dve", lambda e, bT3=bT3: e.tensor_copy(out=brf[:], in_=bT3[:, :, 63]), r=[("ps", bt_)], w=["brf"])
                      P.add("dve", lambda e, bT3=bT3: e.tensor_scalar(out=nbrf[:], in0=bT3[:, :, 63], scalar1=-1.0, scalar2=None, op0=ALU.mult),
                            r=[("ps", bt_)], w=["nbrf"])
                      P.add("act", lambda e, bT3=bT3: e.activation(out=dec2[tt % 2][:], in_=bT3[:, :, 127], func=AF.Exp), r=[("ps", bt_)], w=[("dec", tt % 2)])
                      for blk in range(2):
                          P.add("act", lambda e, blk=blk, bT3=bT3: e.activation(out=eA[:, blk, :], in_=bT3[:, blk, :], func=AF.Exp,
                                                                               bias=nbrf[:, blk:blk + 1], scale=1.0),
                                r=[("ps", bt_), "nbrf"], w=[("eA", blk)])
                          P.add("act", lambda e, blk=blk, bT3=bT3: e.activation(out=eK[:, blk, :], in_=bT3[:, blk, :], func=AF.Exp,
                                                                               bias=brf[:, blk:blk + 1], scale=-1.0),
                                r=[("ps", bt_), "brf"], w=[("eK", blk)])
                      P.add("act", lambda e, bT3=bT3: e.activation(out=eB[:], in_=bT3, func=AF.Exp), r=[("ps", bt_)], w=["eB"])
                      P.add("act", lambda e, bc_=bc_: e.activation(out=eC[:], in_=ps[bc_][:, 0:256], func=AF.Exp), r=[("ps", bc_)], w=["eC"])
                      yield
                      for par in range(2):
                          rs = slice(64 * par, 64 * par + 64)
                          P.add("dve", lambda e, csl=csl, par=par, rs=rs: e.tensor_tensor(out=qAz[par][rs, :, :], in0=gqT[rs, :, csl], in1=eA[rs, :, :], op=ALU.mult),
                                r=[("gqT", 0), ("gqT", 1), ("eA", 0), ("eA", 1)], w=[("qA", par)])
                          P.add("dve", lambda e, csl=csl, par=par, rs=rs: e.tensor_tensor(out=qBz2[tt % 2][par][rs, :, :], in0=gqT[rs, :, csl], in1=eB[rs, :, :], op=ALU.mult),
                                r=[("gqT", 0), ("gqT", 1), "eB"], w=[("qB", tt % 2, par)])
                      P.add("dve", lambda e, csl=csl: e.tensor_tensor(out=kA[:], in0=gkT[:, :, csl], in1=eK[:], op=ALU.mult),
                            r=[("gkT", 0), ("gkT", 1), ("eK", 0), ("eK", 1)], w=["kA"])
                      P.add("dve", lambda e, tt=tt: e.tensor_tensor(out=kC2[tt % 2][:], in0=gktok[:, tt, :], in1=eC[:], op=ALU.mult),
                            r=[("gktok", tt), "eC"], w=[("kC", tt % 2)])
                      yield
                      ba = nbank(ringG)
                      for h in range(4):
                          blk, r0 = h // 2, 64 * (h % 2)
                          P.add("pe", lambda e, ba=ba, h=h, blk=blk, r0=r0: e.matmul(ps[ba][:, h * 128:(h + 1) * 128],
                                                                                     lhsT=kA[:, blk, :], rhs=qAz[h % 2][:, blk, :],
                                                                                     start=True, stop=True),
                                r=["kA", ("qA", h % 2)], w=[("ps", ba)])
                      P.add("dve", lambda e, ba=ba: e.tensor_tensor(out=attT2[tt % 2][:], in0=ps[ba][:, :].rearrange("p (h t) -> p h t", t=128),
                                                                    in1=A(cb, 128, [[288, 128], [0, 4], [1, 128]]), op=ALU.mult),
                            r=[("ps", ba), "cb"], w=[("attT", tt % 2)])
                      yield
                  def gla_back(tt):
                      csl = slice(tt * 128, (tt + 1) * 128)
                      bo = nbank(ringB)
                      for h in range(4):
                          blk, r0 = h // 2, 64 * (h % 2)
                          P.add("pe", lambda e, bo=bo, h=h, tt=tt: e.matmul(ps[bo][:, h * 128:(h + 1) * 128], lhsT=attT2[tt % 2][:, h, :],
                                                                            rhs=gv[:, tt, h * 128:(h + 1) * 128], start=True, stop=False),
                                r=[("attT", tt % 2), ("gv", tt)], w=[("ps", bo)])
                          P.add("pe", lambda e, bo=bo, h=h, blk=blk, r0=r0: e.matmul(ps[bo][:, h * 128:(h + 1) * 128],
                                                                                     lhsT=qBz2[tt % 2][h % 2][:, blk, :], rhs=Sb[:, blk, :],
                                                                                     start=False, stop=True),
                                r=[("qB", tt % 2, h % 2), "Sb"], w=[("ps", bo)])
                      yield
                      bkv = nbank(ringB)
                      for h in range(4):
                          blk, r0 = h // 2, 64 * (h % 2)
                          P.add("pe", lambda e, bkv=bkv, h=h, blk=blk, r0=r0, tt=tt: e.matmul(
                              ps[bkv][r0:r0 + 64, blk * 128:(blk + 1) * 128], lhsT=kC2[tt % 2][:, h * 64:(h + 1) * 64],
                              rhs=gv[:, tt, h * 128:(h + 1) * 128], start=True, stop=True),
                              r=[("kC", tt % 2), ("gv", tt)], w=[("ps", bkv)])
                      for blk in range(2):
                          P.add("dve", lambda e, bkv=bkv, blk=blk: e.scalar_tensor_tensor(
                              out=Sf[:, blk, :], in0=Sf[:, blk, :], scalar=dec2[tt % 2][:, blk:blk + 1], in1=ps[bkv][:, blk * 128:(blk + 1) * 128],
                              op0=ALU.mult, op1=ALU.add), r=[("ps", bkv), ("dec", tt % 2), "Sf"], w=["Sf"])
                      P.add("act", lambda e: e.activation(out=Sb[:], in_=Sf[:], func=AF.Copy), r=["Sf"], w=["Sb"])
                      yield
                      for h in range(4):
                          P.add("act", lambda e, bo=bo, h=h: e.activation(out=junk[:, 0:128], in_=ps[bo][:, h * 128:(h + 1) * 128], func=AF.Square,
                                                                         scale=float(128.0 ** -0.5), accum_out=oms[:, h:h + 1]),
                                r=[("ps", bo)], w=JUNK_RES + ["oms"])
                      P.add("act", lambda e: e.activation(out=orst[:], in_=oms[:], func=AF.Ln, bias=EPS, scale=1.0),
                            r=["oms"], w=["orst"])
                      P.add("act", lambda e: e.activation(out=orst[:], in_=orst[:], func=AF.Exp, scale=-0.5), r=["orst"], w=["orst"])
                      for h in range(4):
                          P.add("dve", lambda e, bo=bo, h=h, tt=tt: e.scalar_tensor_tensor(
                              out=on_[:, h * 128:(h + 1) * 128], in0=ps[bo][:, h * 128:(h + 1) * 128], scalar=orst[:, h:h + 1],
                              in1=t3[:, tt, h * 128:(h + 1) * 128], op0=ALU.mult, op1=ALU.mult),
                              r=[("ps", bo), "orst", ("t3", tt)], w=["on"])
                      yield
                      btr = nbank(ringB)
                      for h in range(4):
                          P.add("pe", lambda e, btr=btr, h=h: e.matmul(ps[btr][:, h * 128:(h + 1) * 128], lhsT=on_[:, h * 128:(h + 1) * 128],
                                                                       rhs=ident_b, start=True, stop=True),
                                r=["on", "cb"], w=[("ps", btr)])
                      P.add("act", lambda e, btr=btr, csl=csl: e.activation(out=attnT[:, 0:4, csl],
                                                                           in_=ps[btr][:, :].rearrange("p (h t) -> p h t", t=128), func=AF.Copy),
                            r=[("ps", btr)], w=[("attnT_g", tt)])
                      yield
                  if T >= 2:
                      for tt_ in range(NT):
                          yield from gla_front(tt_)
                          yield from gla_back(tt_)
                      return
                  yield from gla_front(0)
                  for tt_ in range(NT):
                      gens = [gla_back(tt_)] + ([gla_front(tt_ + 1)] if tt_ + 1 < NT else [])
                      while gens:
                          for g_ in list(gens):
                              try:
                                  next(g_)
                              except StopIteration:
                                  gens.remove(g_)
                          yield
              def moba_gen():
                  P.add("dve", lambda e, T=T: e.tensor_reduce(out=kmsum[:, :, 2 * T:2 * T + 2],
                                                         in_=Kt[:, :, T * TB:(T + 1) * TB].rearrange("p a (j s) -> p a j s", s=256),
                                                         axis=AX.X, op=ALU.add),
                        r=[("Kt", pr, T) for pr in range(4)], w=["kmsum"])
                  P.add("act", lambda e: e.activation(out=kmean[:], in_=kmsum[:], func=AF.Identity, scale=1.0 / 256.0), r=["kmsum"], w=["kmean"])
                  for (src, dst, nm) in ((Qt[:, :, :], qn2, "qn2"), (Kt[:, :, T * TB:(T + 1) * TB], kn2, "kn2")):
                      rr = [("Qt", pr) for pr in range(4)] if nm == "qn2" else [("Kt", pr, T) for pr in range(4)]
                      P.add("act", lambda e, src=src: e.activation(out=sq, in_=src, func=AF.Square), r=rr, w=SQ_RES)
                      bn = nbank(ringM)
                      for pr in range(4):
                          P.add("pe", lambda e, bn=bn, pr=pr: e.matmul(ps[bn][0:8, :], lhsT=cb[:, 256 + pr * 8:264 + pr * 8],
                                                                       rhs=sq[:, pr, :], start=(pr == 0), stop=(pr == 3)),
                                r=SQ_RES + ["cb"], w=[("ps", bn)])
                      P.add("dve", lambda e, bn=bn, dst=dst: e.tensor_reduce(out=dst[:], in_=ps[bn][0:8, :], axis=AX.X, op=ALU.max),
                            r=[("ps", bn)], w=[nm])
                  P.add("dve", lambda e: e.tensor_tensor(out=kn2r[:], in0=kn2r[:], in1=kn2[:], op=ALU.max), r=["kn2", "kn2r"], w=["kn2r"])
                  P.add("dve", lambda e: e.tensor_tensor(out=bnd[:], in0=qn2[:], in1=kn2r[:], op=ALU.mult), r=["qn2", "kn2r"], w=["bnd"])
                  P.add("act", lambda e: e.activation(out=bnd[:], in_=bnd[:], func=AF.Ln, bias=EPS, scale=1.0), r=["bnd"], w=["bnd"])
                  P.add("act", lambda e: e.activation(out=bnd[:], in_=bnd[:], func=AF.Exp, scale=0.5), r=["bnd"], w=["bnd"])
                  P.add("dve", lambda e: e.tensor_scalar(out=bnd8[:], in0=cf[0:8, 1024:1032], scalar1=bnd[:, 0:1], scalar2=None, op0=ALU.mult),
                        r=["bnd", "cf"], w=["bnd8"])
                  bb = nbank(ringM)
                  P.add("pe", lambda e, bb=bb: e.matmul(ps[bb][:, 0:8], lhsT=cf[0:8, 128:256], rhs=bnd8[:], start=True, stop=True),
                        r=["bnd8", "cf"], w=[("ps", bb)])
                  P.add("dve", lambda e, bb=bb: e.tensor_scalar(out=nbias[:], in0=ps[bb][:, 0:8], scalar1=-1.03, scalar2=-0.5,
                                                                op0=ALU.mult, op1=ALU.add), r=[("ps", bb)], w=["nbias"])
                  P.add("dve", lambda e: e.tensor_tensor(out=nbias[:], in0=nbias[:], in1=b31bc[:], op=ALU.add), r=["nbias", "b31bc"], w=["nbias"])

                  need_mask = (T >= 2)
                  if need_mask:
                      for tt in range(NT):
                          own = 2 * T + tt // 2
                          bgs = [nbank(ringM), nbank(ringM)]
                          for h in range(8):
                              pr, r0 = h // 2, 64 * (h % 2)
                              bg = bgs[h % 2]
                              P.add("pe", lambda e, bg=bg, h=h, pr=pr, r0=r0, tt=tt: e.matmul(
                                  ps[bg][:, pr * 8:(pr + 1) * 8], lhsT=Qt[r0:r0 + 64, pr, tt * 128:(tt + 1) * 128],
                                  rhs=kmean[r0:r0 + 64, pr, :], start=True, stop=True),
                                  r=[("Qt", pr), "kmean"], w=[("ps", bg)])
                          for par in range(2):
                              P.add("dve", lambda e, bg=bgs[par], own=own, par=par: e.tensor_tensor(
                                  out=A(gmx, par * 8, [[64, 128], [16, 4], [1, 8]]),
                                  in0=ps[bg][:, 0:32].rearrange("p (a n) -> p a n", n=8),
                                  in1=cf[:, 512 + own * 64:512 + own * 64 + 32].rearrange("p (a n) -> p a n", n=8), op=ALU.add),
                                  r=[("ps", bgs[par]), "cf"], w=["gmx"])
                          for h in range(8):
                              P.add("dve", lambda e, h=h: e.max(out=m8[:, h, :], in_=gmx[:, h * 8:(h + 1) * 8]), r=["gmx"], w=["m8"])
                          P.add("dve", lambda e: e.tensor_tensor(out=selt[:].rearrange("p (h n) -> p h n", n=8),
                                                                 in0=gmx[:].rearrange("p (h n) -> p h n", n=8),
                                                                 in1=A(m8, 3, [[64, 128], [8, 8], [0, 8]]), op=ALU.is_ge),
                                r=["gmx", "m8"], w=["selt"])
                          P.add("dve", lambda e, tt=tt: e.tensor_scalar(out=mrow[:, tt, :], in0=selt[:], scalar1=-1.0, scalar2=BIG,
                                                                        op0=ALU.add, op1=ALU.mult), r=["selt"], w=[("mrow", tt)])

                  LOOK = 2 if T >= 2 else 1
                  tl = []
                  for j in range(i1 + 1):
                      for c in range(2):
                          if j < i0:
                              lo = 0
                          elif j == i0:
                              lo = 128 * c
                          else:
                              lo = 256 + 128 * c
                          tl.append((j, c, lo))
                  items = [(pr, ti, j, c, lo) for pr in range(4) for ti, (j, c, lo) in enumerate(tl)]
                  ntl = len(tl)

                  def mask_prep(pr):
                      bm = nbank(ringM)
                      for par in range(2):
                          h = 2 * pr + par
                          r0 = 64 * par
                          for tt in range(NT):
                              P.add("pe", lambda e, bm=bm, tt=tt, h=h, r0=r0: e.matmul(ps[bm][r0:r0 + 8, tt * 128:(tt + 1) * 128],
                                                                                       lhsT=mrow[:, tt, h * 8:(h + 1) * 8], rhs=ident_b,
                                                                                       start=True, stop=True),
                                    r=[("mrow", tt), "cb"], w=[("ps", bm)])
                      mT = maskT[pr % 2]
                      for par in range(2):
                          r0 = 64 * par
                          P.add("act", lambda e, bm=bm, mT=mT, r0=r0: e.activation(out=mT[r0:r0 + 8, :], in_=ps[bm][r0:r0 + 8, :], func=AF.Copy),
                                r=[("ps", bm)], w=[("maskT", pr % 2, par)])

                  if need_mask:
                      mask_prep(0)
                  yield
                  for idx in range(len(items) + LOOK):
                      if idx < len(items):
                          pr, ti, j, c, lo = items[idx]
                          mT = maskT[pr % 2]
                          if need_mask and ti == 0 and pr + 1 < 4:
                              mask_prep(pr + 1)
                          kt = 2 * j + c
                          use_mask = need_mask and (j < i1)
                          bss = [nbank(ringM), nbank(ringM)]
                          for par in range(2):
                              r0 = 64 * par
                              P.add("pe", lambda e, bs_=bss[par], kt=kt, lo=lo, pr=pr, r0=r0, use_mask=use_mask: e.matmul(
                                  ps[bs_][:, lo:TB], lhsT=Kt[r0:r0 + 64, pr, kt * 128:(kt + 1) * 128], rhs=Qt[r0:r0 + 64, pr, lo:TB],
                                  start=True, stop=(not use_mask)),
                                  r=[("Kt", pr, kt // 4), ("Qt", pr)], w=[("ps", bss[par])])
                          if use_mask:
                              for par in range(2):
                                  r0 = 64 * par
                                  P.add("pe", lambda e, bs_=bss[par], j=j, lo=lo, mT=mT, r0=r0: e.matmul(
                                      ps[bs_][:, lo:TB], lhsT=ohk[r0:r0 + 8, j * 128:(j + 1) * 128], rhs=mT[r0:r0 + 8, lo:TB], start=False, stop=True),
                                      r=["ohk", "ohk2", ("maskT", pr % 2, par)], w=[("ps", bss[par])])
                          for par in range(2):
                              h = 2 * pr + par
                              pi = (2 * idx + par) % NPT
                              pt = Pt[pi]
                              pres = ("Pt", pi)
                              P.add("act", lambda e, bs_=bss[par], lo=lo, pt=pt, h=h: e.activation(out=pt[:, lo:TB], in_=ps[bs_][:, lo:TB], func=AF.Exp,
                                                                                                 bias=nbias[:, h:h + 1], scale=1.0),
                                    r=[("ps", bss[par]), "nbias"], w=[pres])
                              fix = []
                              if j == i0:
                                  fix.append((lo, 256, Mown[:, h, 0:256 - lo]))
                                  if c == 1:
                                      fix.append((256, 368, Mprev[:, h, :]))
                              elif j == i1:
                                  fix.append((lo, 512, Mown[:, h, 0:512 - lo]))
                              elif j == i0 - 1 and c == 1:
                                  fix.append((0, 112, Mprev[:, h, :]))
                              for (a0, a1, tab) in fix:
                                  P.add("dve", lambda e, pt=pt, a0=a0, a1=a1, tab=tab: e.tensor_tensor(out=pt[:, a0:a1], in0=pt[:, a0:a1], in1=tab,
                                                                                                      op=ALU.mult),
                                        r=[pres, ("Mown", h), ("Mprev", h)], w=[pres])
                      if idx - LOOK >= 0:
                          pr, ti, j, c, lo = items[idx - LOOK]
                          kt = 2 * j + c
                          for par in range(2):
                              h = 2 * pr + par
                              r0 = 64 * par
                              dr0 = 64 - r0
                              acc = 6 + par
                              pi = (2 * (idx - LOOK) + par) % NPT
                              pt = Pt[pi]
                              pres = ("Pt", pi)
                              vcol = pr * 192 + (0 if par == 0 else 64)
                              P.add("pe", lambda e, acc=acc, kt=kt, lo=lo, pt=pt, vcol=vcol, ti=ti, ntl=ntl: e.matmul(
                                  ps[acc][:, lo:TB], lhsT=Vint[:, kt, vcol:vcol + 128], rhs=pt[:, lo:TB],
                                  start=(ti == 0), stop=(ti == ntl - 1), skip_group_check=True),
                                  r=[pres, ("V", kt)], w=[("ps", acc)])
                              if ti == ntl - 1:
                                  rd = rden2[par]
                                  rres = ("rden", par)
                                  P.add("dve", lambda e, acc=acc, r0=r0, dr0=dr0, rd=rd: e.reciprocal(out=rd[r0:r0 + 64, :], in_=ps[acc][dr0:dr0 + 64, :]),
                                        r=[("ps", acc)], w=[rres])
                                  P.add("dve", lambda e, acc=acc, r0=r0, pr=pr, rd=rd: e.tensor_tensor(out=attnT[r0:r0 + 64, 4 + pr, :], in0=ps[acc][r0:r0 + 64, :],
                                                                                                in1=rd[r0:r0 + 64, :], op=ALU.mult),
                                        r=[("ps", acc), rres], w=[("attnT_m", h)])
                      yield
              if T < 2:
                  ringG.banks, ringB.banks, ringM.banks = [0], [1, 2], [3, 4, 5]
              else:
                  ringG.banks, ringB.banks, ringM.banks = [0], [0, 1], [2, 3, 4, 5]
              ringG.reset()
              ringB.reset()
              ringM.reset()
              gg_ = gla_gen()
              mg_ = moba_gen()
              ratio = max(1, int(round(8.0 * (i1 + 1) / 36.0)))
              alive_g, alive_m = True, True
              while alive_g or alive_m:
                  if alive_g:
                      try:
                          next(gg_)
                      except StopIteration:
                          alive_g = False
                  for _ in range(ratio if alive_g else 4):
                      if alive_m:
                          try:
                              next(mg_)
                          except StopIteration:
                              alive_m = False
              if isdbg:
                  dump("attnT_g", attnT[:, 0:4, :], [("attnT_g", t_) for t_ in range(NT)])
              if isdbg:
                  dump("attnT_m", attnT[:, 4:8, :], [("attnT_m", h_) for h_ in range(8)])
              if stop_after == "moba":
                  break

              ATT_ALL = [("attnT_g", tt) for tt in range(NT)] + [("attnT_m", h) for h in range(8)]
              wvs = [wload(wb_out_v[:, :, ch * 512:(ch + 1) * 512], "wb_out", 8, 512) for ch in range(2)]
              for tt in range(NT):
                  for ch in range(2):
                      wv, wr = wvs[ch]
                      bk = nbank()
                      acc_mm(ps[bk][:, :], [(attnT[:, kc, tt * 128:(tt + 1) * 128], wv[:, kc, :]) for kc in range(8)],
                             r=[wr] + ATT_ALL, w=[("ps", bk)])
                      P.add("dve", lambda e, bk=bk, ch=ch: e.tensor_tensor(out=sig[:], in0=ps[bk][:, :], in1=gate_a[:, ch * 512:(ch + 1) * 512],
                                                                           op=ALU.mult), r=[("ps", bk), ("gate_a", ch)], w=["sig"])
                      P.add("dve", lambda e, tt=tt, ch=ch: e.tensor_tensor(out=xblk[:, tt, ch * 512:(ch + 1) * 512],
                                                                           in0=xblk[:, tt, ch * 512:(ch + 1) * 512], in1=sig[:], op=ALU.add),
                            r=["sig", ("x", tt)], w=[("x", tt)])
              if isdbg:
                  dump("x1", xblk[:].rearrange("p t d -> p (t d)"), [("x", t_) for t_ in range(NT)])
              norm_hT(b, 1)
              for pc in range(8):
                  wv, wr = wload(wb_1_v[:, :, pc * 512:(pc + 1) * 512], "wb_1", 8, 512)
                  for fl in range(4):
                      fc = pc * 4 + fl
                      bk = nbank()
                      acc_mm(ps[bk][:, :], [(wv[:, kc, fl * 128:(fl + 1) * 128], hT[:, kc, :]) for kc in range(8)],
                             r=[wr] + HT_ALL, w=[("ps", bk)])
                      P.add("act", lambda e, bk=bk, fc=fc: e.activation(out=uT[:, fc, :], in_=ps[bk][:, :], func=AF.Relu),
                            r=[("ps", bk)], w=[("uT", fc)])
                      P.add("dve", lambda e, fc=fc: e.tensor_tensor(out=uT[:, fc, :], in0=uT[:, fc, :], in1=uT[:, fc, :], op=ALU.mult),
                            r=[("uT", fc)], w=[("uT", fc)])
              for pc in range(8):
                  wv, wr = wload(wb_2_v[:, pc * 4:(pc + 1) * 4, :], "wb_2", 4, 1024)
                  order = [(fl, tt, ch) for fl in range(4) for tt in range(NT) for ch in range(2)]
                  if pc == 7:
                      order = [(fl, tt, ch) for tt in range(NT) for ch in range(2) for fl in range(4)]
                  for (fl, tt, ch) in order:
                      fc = pc * 4 + fl
                      if True:
                          if True:
                              bk = tt * 2 + ch
                              P.add("pe", lambda e, bk=bk, fc=fc, fl=fl, tt=tt, ch=ch, wv=wv: e.matmul(
                                  ps[bk][:, :], lhsT=uT[:, fc, tt * 128:(tt + 1) * 128], rhs=wv[:, fl, ch * 512:(ch + 1) * 512],
                                  start=(fc == 0), stop=(fc == 31), skip_group_check=True),
                                  r=[wr, ("uT", fc)], w=[("ps", bk)])
              for tt in range(NT):
                  for ch in range(2):
                      bk = tt * 2 + ch
                      P.add("dve", lambda e, bk=bk, ch=ch: e.tensor_tensor(out=sig[:], in0=ps[bk][:, :], in1=gate_m[:, ch * 512:(ch + 1) * 512],
                                                                           op=ALU.mult), r=[("ps", bk), ("gate_m", ch)], w=["sig"])
                      P.add("dve", lambda e, tt=tt, ch=ch: e.tensor_tensor(out=xblk[:, tt, ch * 512:(ch + 1) * 512],
                                                                           in0=xblk[:, tt, ch * 512:(ch + 1) * 512], in1=sig[:], op=ALU.add),
                            r=["sig", ("x", tt)], w=[("x", tt)])
              norm_stats()
              for tt in range(NT):
                  P.add("dve", lambda e, tt=tt: e.scalar_tensor_tensor(out=xblk[:, tt, :], in0=xblk[:, tt, :], scalar=rstd[:, tt:tt + 1],
                                                                       in1=gfin_bc[:], op0=ALU.mult, op1=ALU.mult),
                        r=[("x", tt), ("rstd", tt), "gfin_bc"], w=[("x", tt)])
                  odst = out_d.ap()[b, T * TB + tt * 128:T * TB + (tt + 1) * 128, :]
                  op = P.add("sp", lambda e, odst=odst, tt=tt: e.dma_start(out=odst, in_=xblk[:, tt, :]), r=[("x", tt)],
                             w=[("out", b, T, tt)], dma_key=("ostore", tt))
                  out_ops.append(op)
          if stop_after is not None:
              break
    except _Stop:
        pass
    if not out_ops:
        op = P.add("sp", lambda e: e.dma_start(out=out_d.ap()[0, 0:TB, :].rearrange("(t p) d -> p t d", p=128), in_=xblk[:]),
                   r=[("x", t_) for t_ in range(NT)], w=[("out", 0, 0)], dma_key="ostore")
        out_ops.append(op)
    fin = [out_ops[-1]]
    for o in P.ops:
        if o.dma_key is not None and isinstance(o.dma_key, tuple) and o.dma_key[0] == "dbg":
            fin.append(o)
    P.finals = fin
    return nc, P


_CACHE = {}


def make_inputs_common(inputs):
    cs = host_consts()
    com = {
        "w_ada": np.ascontiguousarray(inputs["w_ada"][0], dtype=np.float32),
        "b_ada": np.ascontiguousarray(inputs["b_ada"][0].reshape(48, 128), dtype=np.float32),
        "g_mix": np.ascontiguousarray(inputs["g_mix"][0].reshape(8, 128), dtype=np.float32),
        "g_mlp": np.ascontiguousarray(inputs["g_mlp"][0].reshape(8, 128), dtype=np.float32),
        "w_in": np.ascontiguousarray(inputs["w_in"][0], dtype=np.float32),
        "w_gla_gate": np.ascontiguousarray(inputs["w_gla_gate"][0], dtype=np.float32),
        "b_gla_gate": np.ascontiguousarray(inputs["b_gla_gate"][0].reshape(1, 256), dtype=np.float32),
        "g_gla_out": np.ascontiguousarray(inputs["g_gla_out"][0].reshape(1, 512), dtype=np.float32),
        "rel_bias": np.ascontiguousarray(inputs["rel_bias"], dtype=np.float32),
        "w_out": np.ascontiguousarray(inputs["w_out"][0], dtype=np.float32),
        "w_ff1": np.ascontiguousarray(inputs["w_ff1"][0], dtype=np.float32),
        "w_ff2": np.ascontiguousarray(inputs["w_ff2"][0], dtype=np.float32),
        "g_final": np.ascontiguousarray(inputs["g_final"].reshape(1, D), dtype=np.float32),
    }
    com.update(cs)
    return com


def kernel(**inputs):
    x = np.asarray(inputs["x"], dtype=np.float32)
    c = np.asarray(inputs["c"], dtype=np.float32)
    com = make_inputs_common(inputs)
    nc, P = build_nc(SEQ_PER_CORE, NBLK)
    P.emit(nc)
    in_maps = []
    for k in range(NCORES):
        m = dict(com)
        m["x"] = np.ascontiguousarray(x[k * SEQ_PER_CORE:(k + 1) * SEQ_PER_CORE])
        m["c"] = np.ascontiguousarray(c[k * SEQ_PER_CORE:(k + 1) * SEQ_PER_CORE])
        in_maps.append(m)
    res = run_bass_kernel_spmd(nc, in_maps, core_ids=list(range(NCORES)))
    out = np.concatenate([np.asarray(r["out"], dtype=np.float32) for r in res.results], axis=0)
    return out
```
